# Optimizing a Trainium2 kernel written in Bass

```python
import math
import jax, jax.numpy as jnp
from jax import lax
import numpy as np

D_MODEL = 2048
BATCH = 4
SEQ = 4096
DEPTH = 1

EPS = 1e-6
BLOCK = 128
NEG_INF = -1e30

MLA_HEADS = 8
MLA_NOPE = 128
MLA_ROPE = 64
MLA_V = 128
MLA_Q_RANK = 512
MLA_KV_RANK = 256
MLA_QK = MLA_NOPE + MLA_ROPE
ROPE_THETA = 10000.0

SWA_HEADS = 16
SWA_KV_HEADS = 2
SWA_HD = 64
SWA_GROUP = SWA_HEADS // SWA_KV_HEADS
WINDOW = 128

N_BUCKETS = 32
MAX_DISTANCE = 128

PEER_HEADS = 8
PEER_NKEYS = 128
PEER_N = PEER_NKEYS * PEER_NKEYS
PEER_DQ = 256
PEER_TOPK = 16
PEER_CHUNK = 64

MLA_OUT = MLA_HEADS * MLA_V
SWA_OUT = SWA_HEADS * SWA_HD
MIX_WIDTH = MLA_OUT + SWA_OUT
IN_SPLITS = (MLA_Q_RANK, MLA_KV_RANK, MLA_ROPE, SWA_HEADS * SWA_HD, SWA_KV_HEADS * SWA_HD, SWA_KV_HEADS * SWA_HD)
IN_COLS = sum(IN_SPLITS)

kernel_name = "hybrid_mla_swa_peer_block"


def rms_norm(x, g):
    xf = x.astype(jnp.float32)
    y = xf * lax.rsqrt(jnp.mean(xf * xf, axis=-1, keepdims=True) + EPS)
    return (y * g.astype(jnp.float32)).astype(x.dtype)


def rope(x, positions):
    half = x.shape[-1] // 2
    inv_freq = ROPE_THETA ** (-jnp.arange(half, dtype=jnp.float32) / half)
    ang = positions.astype(jnp.float32)[:, :, None, None] * inv_freq
    cos, sin = jnp.cos(ang), jnp.sin(ang)
    x1 = x[..., :half].astype(jnp.float32)
    x2 = x[..., half:].astype(jnp.float32)
    out = jnp.concatenate([x1 * cos - x2 * sin, x2 * cos + x1 * sin], axis=-1)
    return out.astype(x.dtype)


def t5_bucket(dist):
    n = jnp.maximum(dist, 0)
    max_exact = N_BUCKETS // 2
    nf = jnp.maximum(n, 1).astype(jnp.float32)
    large = max_exact + (jnp.log(nf / max_exact) / math.log(MAX_DISTANCE / max_exact)
                         * (N_BUCKETS - max_exact)).astype(jnp.int32)
    large = jnp.minimum(large, N_BUCKETS - 1)
    return jnp.where(n < max_exact, n, large)


def mla_group(q_lat, kv_lat, k_pe, positions, q_a_gain, w_q_b, kv_a_gain, w_kv_b, q_gain, k_gain):
    B, S, _ = q_lat.shape
    q = (rms_norm(q_lat, q_a_gain) @ w_q_b).reshape(B, S, MLA_HEADS, MLA_QK)
    kv = (rms_norm(kv_lat, kv_a_gain) @ w_kv_b).reshape(B, S, MLA_HEADS, MLA_NOPE + MLA_V)
    k_nope, v = kv[..., :MLA_NOPE], kv[..., MLA_NOPE:]
    k = jnp.concatenate([k_nope, jnp.broadcast_to(k_pe[:, :, None, :], (B, S, MLA_HEADS, MLA_ROPE))], axis=-1)
    q = rms_norm(q, q_gain)
    k = rms_norm(k, k_gain)
    q = jnp.concatenate([q[..., :MLA_NOPE], rope(q[..., MLA_NOPE:], positions)], axis=-1)
    k = jnp.concatenate([k[..., :MLA_NOPE], rope(k[..., MLA_NOPE:], positions)], axis=-1)
    scale = MLA_QK ** -0.5
    nb = S // BLOCK
    q_blocks = q.reshape(B, nb, BLOCK, MLA_HEADS, MLA_QK).transpose(1, 0, 2, 3, 4)
    k_idx = jnp.arange(S)

    def one_block(args):
        qb, bi = args
        s = jnp.einsum('bqhd,bkhd->bhqk', qb, k, preferred_element_type=jnp.float32) * scale
        q_idx = bi * BLOCK + jnp.arange(BLOCK)
        causal = k_idx[None, :] <= q_idx[:, None]
        s = jnp.where(causal[None, None], s, NEG_INF)
        p = jax.nn.softmax(s, axis=-1).astype(v.dtype)
        return jnp.einsum('bhqk,bkhd->bqhd', p, v)

    o = lax.map(one_block, (q_blocks, jnp.arange(nb)))
    return o.transpose(1, 0, 2, 3, 4).reshape(B, S, MLA_OUT)


def band(t, nb):
    B, S = t.shape[:2]
    rest = t.shape[2:]
    tp = jnp.pad(t, [(0, 0), (BLOCK, 0)] + [(0, 0)] * len(rest))
    prev = tp[:, :S].reshape(B, nb, BLOCK, *rest)
    cur = t.reshape(B, nb, BLOCK, *rest)
    return jnp.concatenate([prev, cur], axis=2)


def swa_group(q, k, v, positions, q_gain, k_gain, sinks, rel_table):
    B, S, _ = q.shape
    nb = S // BLOCK
    q = rms_norm(q.reshape(B, S, SWA_KV_HEADS, SWA_GROUP, SWA_HD), q_gain)
    k = rms_norm(k.reshape(B, S, SWA_KV_HEADS, SWA_HD), k_gain)
    v = v.reshape(B, S, SWA_KV_HEADS, SWA_HD)
    qb = q.reshape(B, nb, BLOCK, SWA_KV_HEADS, SWA_GROUP, SWA_HD)
    kb, vb, pb = band(k, nb), band(v, nb), band(positions, nb)
    s = jnp.einsum('bnqkgd,bnjkd->bnkgqj', qb, kb, preferred_element_type=jnp.float32) * (SWA_HD ** -0.5)
    qp = positions.reshape(B, nb, BLOCK)
    bucket = t5_bucket(qp[..., :, None] - pb[..., None, :])
    bias = rel_table[bucket].astype(jnp.float32)
    bias = bias.reshape(B, nb, BLOCK, 2 * BLOCK, SWA_KV_HEADS, SWA_GROUP).transpose(0, 1, 4, 5, 2, 3)
    s = s + bias
    q_idx = jnp.arange(S).reshape(nb, BLOCK)
    k_idx = (jnp.arange(nb) * BLOCK - BLOCK)[:, None] + jnp.arange(2 * BLOCK)[None, :]
    off = q_idx[:, :, None] - k_idx[:, None, :]
    mask = (off >= 0) & (off < WINDOW) & (k_idx[:, None, :] >= 0)
    s = jnp.where(mask[None, :, None, None], s, NEG_INF)
    sink = jnp.broadcast_to(sinks.astype(jnp.float32).reshape(SWA_KV_HEADS, SWA_GROUP)[None, None, :, :, None, None],
                            s.shape[:-1] + (1,))
    p = jax.nn.softmax(jnp.concatenate([s, sink], axis=-1), axis=-1)[..., :-1].astype(vb.dtype)
    o = jnp.einsum('bnkgqj,bnjkd->bnqkgd', p, vb)
    return o.reshape(B, S, SWA_OUT)


def peer(h, w_q, sub_keys, expert_u, expert_v):
    B, S, D = h.shape
    T = B * S
    xt = h.reshape(T, D)
    q = (xt @ w_q).reshape(T, PEER_HEADS, 2, PEER_DQ // 2)
    s = jnp.einsum('thpd,hpnd->thpn', q, sub_keys, preferred_element_type=jnp.float32)
    s_half, i_half = lax.top_k(s, PEER_TOPK)
    cand = (s_half[:, :, 0, :, None] + s_half[:, :, 1, None, :]).reshape(T, PEER_HEADS, PEER_TOPK * PEER_TOPK)
    cand_idx = (i_half[:, :, 0, :, None] * PEER_NKEYS + i_half[:, :, 1, None, :]).reshape(T, PEER_HEADS, PEER_TOPK * PEER_TOPK)
    top_s, pos = lax.top_k(cand, PEER_TOPK)
    idx = jnp.take_along_axis(cand_idx, pos, axis=-1)
    g = jax.nn.softmax(top_s, axis=-1)
    n_chunks = T // PEER_CHUNK
    HK = PEER_HEADS * PEER_TOPK

    def one_chunk(args):
        xc, ic, gc = args
        u = expert_u[ic]
        a = jnp.einsum('cd,ced->ce', xc, u, preferred_element_type=jnp.float32)
        w = (gc * jax.nn.gelu(a)).astype(xc.dtype)
        return jnp.einsum('ce,ced->cd', w, expert_v[ic])

    y = lax.map(one_chunk, (xt.reshape(n_chunks, PEER_CHUNK, D),
                            idx.reshape(n_chunks, PEER_CHUNK, HK),
                            g.reshape(n_chunks, PEER_CHUNK, HK)))
    return y.reshape(B, S, D)


def setup_inputs(seed: int = 0) -> dict:
    key = jax.random.key(seed)
    ks = jax.random.split(key, 24)
    f32 = jnp.float32
    L = DEPTH

    def nrm(k, shape, scale):
        return jax.random.normal(k, shape, f32) * scale

    def gain(k, shape):
        return 1.0 + 0.05 * jax.random.normal(k, shape, f32)

    x = jax.random.normal(ks[0], (BATCH, SEQ, D_MODEL), f32)
    offset = jax.random.randint(ks[1], (BATCH, 1), 0, 1024, dtype=jnp.int32)
    positions = (offset + jnp.arange(SEQ, dtype=jnp.int32)[None, :]).astype(jnp.int32)
    return {
        "x": x,
        "positions": positions,
        "norm1_gain": gain(ks[2], (L, D_MODEL)),
        "w_in": nrm(ks[3], (L, D_MODEL, IN_COLS), D_MODEL ** -0.5),
        "q_a_gain": gain(ks[4], (L, MLA_Q_RANK)),
        "w_q_b": nrm(ks[5], (L, MLA_Q_RANK, MLA_HEADS * MLA_QK), MLA_Q_RANK ** -0.5),
        "kv_a_gain": gain(ks[6], (L, MLA_KV_RANK)),
        "w_kv_b": nrm(ks[7], (L, MLA_KV_RANK, MLA_HEADS * (MLA_NOPE + MLA_V)), MLA_KV_RANK ** -0.5),
        "mla_q_gain": gain(ks[8], (L, MLA_QK)),
        "mla_k_gain": gain(ks[9], (L, MLA_QK)),
        "swa_q_gain": gain(ks[10], (L, SWA_HD)),
        "swa_k_gain": gain(ks[11], (L, SWA_HD)),
        "swa_sinks": nrm(ks[12], (L, SWA_HEADS), 1.0),
        "rel_bias_table": nrm(ks[13], (N_BUCKETS, SWA_HEADS), 0.5),
        "group_out_gain": gain(ks[14], (L, MIX_WIDTH)),
        "w_out": nrm(ks[15], (L, MIX_WIDTH, D_MODEL), MIX_WIDTH ** -0.5),
        "norm2_gain": gain(ks[16], (L, D_MODEL)),
        "peer_w_q": nrm(ks[17], (L, D_MODEL, PEER_HEADS * PEER_DQ), D_MODEL ** -0.5),
        "peer_sub_keys": nrm(ks[18], (L, PEER_HEADS, 2, PEER_NKEYS, PEER_DQ // 2), (PEER_DQ // 2) ** -0.5),
        "peer_u": nrm(ks[19], (L, PEER_N, D_MODEL), D_MODEL ** -0.5),
        "peer_v": nrm(ks[20], (L, PEER_N, D_MODEL), 0.3),
    }


def reference(x, positions, norm1_gain, w_in, q_a_gain, w_q_b, kv_a_gain, w_kv_b, mla_q_gain, mla_k_gain,
              swa_q_gain, swa_k_gain, swa_sinks, rel_bias_table, group_out_gain, w_out, norm2_gain,
              peer_w_q, peer_sub_keys, peer_u, peer_v):
    offsets = np.cumsum(IN_SPLITS)[:-1].tolist()
    h = x
    for l in range(DEPTH):
        n1 = rms_norm(h, norm1_gain[l])
        proj = n1 @ w_in[l]
        q_lat, kv_lat, k_pe, q_swa, k_swa, v_swa = jnp.split(proj, offsets, axis=-1)
        o_mla = mla_group(q_lat, kv_lat, k_pe, positions, q_a_gain[l], w_q_b[l], kv_a_gain[l], w_kv_b[l],
                          mla_q_gain[l], mla_k_gain[l])
        o_swa = swa_group(q_swa, k_swa, v_swa, positions, swa_q_gain[l], swa_k_gain[l], swa_sinks[l],
                          rel_bias_table)
        g_out = group_out_gain[l]
        mixed = jnp.concatenate([rms_norm(o_mla, g_out[:MLA_OUT]), rms_norm(o_swa, g_out[MLA_OUT:])], axis=-1)
        h = h + mixed @ w_out[l]
        h = h + peer(rms_norm(h, norm2_gain[l]), peer_w_q[l], peer_sub_keys[l], peer_u[l], peer_v[l])
    return h
```

```python
import math
import numpy as np
import concourse.bass as bass
import concourse.mybir as mybir
from concourse.bass_utils import run_bass_kernel_spmd
from contextlib import ExitStack

F32 = mybir.dt.float32
BF16 = mybir.dt.bfloat16
I32 = mybir.dt.int32
ALU = mybir.AluOpType
AF = mybir.ActivationFunctionType
AX = mybir.AxisListType


def _is_ap(v):
    return hasattr(v, "partition_size") and hasattr(v, "rearrange")


class Sched:
    ENG = ("pe", "dve", "act", "pool", "sp")

    def __init__(self, nc, es, n_dma_sems=48):
        self.nc = nc
        self.es = es
        self.q = {k: [] for k in self.ENG}
        self.sems = []
        self.esem = {}
        for k in ("pe", "dve", "act", "pool"):
            self.esem[k] = len(self.sems)
            self.sems.append(es.enter_context(nc.semaphore("s_" + k)))
        self.cnt = {k: 0 for k in self.esem}
        self.dma_ids = []
        self.dma_pool = {"sp": [], "pool": []}
        for i in range(n_dma_sems):
            self.dma_ids.append(len(self.sems))
            self.dma_pool["sp" if i < (2 * n_dma_sems) // 3 else "pool"].append(len(self.sems))
            self.sems.append(es.enter_context(nc.semaphore("s_dma%d" % i)))
        self.dma_cnt = {i: 0 for i in self.dma_ids}
        self.dma_rr = {"sp": 0, "pool": 0}
        self.waited = {}
        self.last_w = {}
        self.readers = {}
        self.barrier_tok = {}
        self.n_wait = 0

    def sb(self, name, shape, dtype, es=None):
        return (es or self.es).enter_context(self.nc.sbuf_tensor(name, list(shape), dtype))

    def ps(self, name, shape, dtype=F32, es=None):
        return (es or self.es).enter_context(self.nc.psum_tensor(name, list(shape), dtype))

    def barrier(self):
        tok = {}
        for k in ("pe", "dve", "act", "pool"):
            if self.cnt[k] > 0:
                tok[self.esem[k]] = self.cnt[k]
        for si in self.dma_ids:
            if self.dma_cnt[si] > 0:
                tok[si] = 16 * self.dma_cnt[si]
        self.barrier_tok = tok

    def _record(self, eng, fn, reads, writes, dma=False):
        need = dict(self.barrier_tok)

        def add(tok):
            if tok is None:
                return
            s, v = tok
            if need.get(s, 0) < v:
                need[s] = v

        for k in reads:
            add(self.last_w.get(k))
        for k in writes:
            add(self.last_w.get(k))
            for t in self.readers.get(k, ()):
                add(t)
        if dma:
            pool_ = self.dma_pool[eng]
            si = pool_[self.dma_rr[eng] % len(pool_)]
            self.dma_rr[eng] += 1
            if self.dma_cnt[si] > 0:
                add((si, 16 * self.dma_cnt[si]))
            self.dma_cnt[si] += 1
            tok = (si, 16 * self.dma_cnt[si])
            inc = (si, 16)
        else:
            self.cnt[eng] += 1
            tok = (self.esem[eng], self.cnt[eng])
            inc = (self.esem[eng], 1)
        waits = []
        for s, v in need.items():
            if eng == "pe" and s == self.esem["pe"]:
                continue
            if self.waited.get((eng, s), 0) >= v:
                continue
            self.waited[(eng, s)] = v
            waits.append((s, v))
        self.n_wait += len(waits)
        self.q[eng].append((waits, fn, inc))
        for k in writes:
            self.last_w[k] = tok
            self.readers[k] = []
        for k in reads:
            self.readers.setdefault(k, []).append(tok)
        return tok

    def op(self, eng, meth, *, r=None, w=None, xr=(), xw=(), **kw):
        reads, writes = [], []
        for k, v in kw.items():
            if _is_ap(v):
                if k in ("out", "accum_out", "ap"):
                    writes.append(v.name)
                else:
                    reads.append(v.name)
        if r is not None:
            reads = list(r)
        if w is not None:
            writes = list(w)
        reads += list(xr)
        writes += list(xw)

        def fn(e, meth=meth, kw=kw):
            return getattr(e, meth)(**kw)

        return self._record(eng, fn, reads, writes)

    def dma(self, eng, out, in_, r=None, w=None, **kw):
        reads = [in_.name] if r is None else list(r)
        writes = [out.name] if w is None else list(w)

        def fn(e, out=out, in_=in_, kw=kw):
            return e.dma_start(out=out, in_=in_, **kw)

        return self._record(eng, fn, reads, writes, dma=True)

    def emit(self):
        final_waits = []
        for si in self.dma_ids:
            if self.dma_cnt[si] > 0:
                final_waits.append((si, 16 * self.dma_cnt[si]))
        for k in ("pe", "dve", "act", "pool"):
            if self.cnt[k] > 0:
                final_waits.append((self.esem[k], self.cnt[k]))
        sems = self.sems
        q = self.q

        def mk(name):
            def body(e):
                for waits, fn, inc in q[name]:
                    for s, v in waits:
                        e.wait_ge(sems[s], v)
                    ins = fn(e)
                    ins.then_inc(sems[inc[0]], inc[1])
                if name == "sp":
                    for s, v in final_waits:
                        e.wait_ge(sems[s], v)
            return body

        with self.nc.Block() as block:
            block.tensor(mk("pe"))
            block.vector(mk("dve"))
            block.scalar(mk("act"))
            block.gpsimd(mk("pool"))
            block.sync(mk("sp"))


D = 2048
NS = 32
NT = 16
EPS = 1e-6
TWO_PI = 2.0 * math.pi
C1 = 6.28125
C2 = TWO_PI - C1
NEG = -30000.0
T5_THR = [float(b) for b in range(1, 17)] + [float(math.ceil(16.0 * 8.0 ** (k / 16.0))) for k in range(1, 16)]
NB = 4
NCHUNK = 128
NGRP = 4
DELTA = 1e-5


def build_program(upto="all", dbg_names=(), ngrp=NGRP, nblk=NCHUNK // NB):
    nc = bass.Bass("TRN2", target_bir_lowering=False)

    def din(name, shape, dt=F32):
        return nc.dram_tensor(name, list(shape), dt, kind="ExternalInput").ap()

    def dscr(name, shape, dt=BF16):
        return nc.dram_tensor(name, list(shape), dt, kind="Internal").ap()

    x_s = din("x_s", [NS, 128, D])
    pos_s = din("pos_s", [128, NS], I32)
    pos_r = din("pos_r", [NS, 128], I32)
    flags = din("flags", [128, 2])
    invf = din("invf", [128, 32])
    g1 = din("norm1_gain", [D])
    w_in = din("w_in", [D, 2112])
    qag = din("q_a_gain", [512])
    w_qb = din("w_q_b", [512, 1536])
    kvag = din("kv_a_gain", [256])
    w_kvb = din("w_kv_b", [256, 2048])
    mqg = din("mla_q_gain", [192])
    mkg = din("mla_k_gain", [192])
    sqg = din("swa_q_gain", [64])
    skg = din("swa_k_gain", [64])
    sinks = din("swa_sinks", [16])
    relt = din("rel_bias_table", [32 * 16])
    gog = din("group_out_gain", [D])
    w_out = din("w_out", [D, D])
    g2 = din("norm2_gain", [D])
    pwq = din("peer_w_q", [D, D])
    skT_d = din("skT", [128, 16, 128])
    ut_t = din("ut_t", [NCHUNK, 128, 16, 128])
    pv = din("peer_v", [128 * NCHUNK, D])
    out_h = nc.dram_tensor("out", [NT, 128, D], F32, kind="ExternalOutput").ap()

    kt_scr = dscr("kt_scr", [8, NS, 128, 128])
    kpe_scr = dscr("kpe_scr", [4, NS, 128, 128])
    v_scr = dscr("v_scr", [NS, 128, 8, 129])
    qt_scr = dscr("qt_scr", [8, NT, 128, 128])
    qpe_scr = dscr("qpe_scr", [4, NT, 128, 128])
    mix_scr = dscr("mix_scr", [NT, 128, 8, 128])
    hn_scr = dscr("hn_scr", [NGRP, 128, 16, 4, 128])
    q_scr = dscr("q_scr", [NGRP, 128, 16, 512])

    dbg_out = {}

    with ExitStack() as es:
        s = Sched(nc, es)

        def dbg(name, ap, shape, dt=F32):
            if name in dbg_names:
                t = nc.dram_tensor("dbg_" + name, list(shape), dt, kind="ExternalOutput").ap()
                s.dma("sp", t, ap)

        def bcast_load(dst, src_1d, n, eng="sp"):
            s.dma(eng, dst, src_1d.rearrange("(o n) -> o n", o=1).to_broadcast([128, n]))

        ident = s.sb("ident", [128, 128], BF16)
        tri = s.sb("tri", [128, 128], BF16)
        trif = s.sb("trif", [128, 128], F32)
        epsT = s.sb("epsT", [128, 1], F32)
        lnps = s.sb("lnps", [128, 1], F32)
        mhalf = s.sb("mhalf", [128, 16], F32)
        flg = s.sb("flg", [128, 2], F32)
        phA = ExitStack()
        cosT = s.sb("cosT", [128, NS, 32], F32, phA)
        sinT = s.sb("sinT", [128, NS, 32], F32, phA)
        posf = s.sb("posf", [128, NS], F32, phA)
        swa_kt = s.sb("swa_kt", [128, NS * 128], BF16, phA)
        swa_v = s.sb("swa_v", [128, NS, 2, 65], BF16, phA)

        with ExitStack() as ph:
            it = s.sb("it", [128, 128], F32, ph)
            s.op("pool", "iota", out=it[:], pattern=[[1, 128]], base=0, channel_multiplier=-1,
                 allow_small_or_imprecise_dtypes=True)
            s.op("dve", "tensor_single_scalar", out=trif[:], in_=it[:], scalar=0.0, op=ALU.is_equal)
            s.op("dve", "tensor_copy", out=ident[:], in_=trif[:])
            s.op("dve", "tensor_single_scalar", out=trif[:], in_=it[:], scalar=0.0, op=ALU.is_ge)
            s.op("dve", "tensor_copy", out=tri[:], in_=trif[:])
            s.op("dve", "memset", ap=epsT[:], constant=EPS)
            s.op("dve", "memset", ap=lnps[:], constant=math.log(192.0 ** -0.5))
            s.op("dve", "memset", ap=mhalf[:], constant=-0.5)
            s.op("dve", "memset", ap=swa_v[:], constant=1.0)
            s.dma("sp", flg[:], flags)
            posi = s.sb("posi", [128, NS], I32, ph)
            s.dma("sp", posi[:], pos_s)
            s.op("dve", "tensor_copy", out=posf[:], in_=posi[:])
            ivf = s.sb("ivf", [128, 32], F32, ph)
            s.dma("sp", ivf[:], invf)
            ang = s.sb("ang", [128, NS, 32], F32, ph)
            s.op("dve", "tensor_tensor", out=ang[:], in0=posf[:].unsqueeze(2).to_broadcast([128, NS, 32]),
                 in1=ivf[:].unsqueeze(1).to_broadcast([128, NS, 32]), op=ALU.mult)
            kq = s.sb("kq", [128, NS, 32], F32, ph)
            ki = s.sb("ki", [128, NS, 32], I32, ph)
            red = s.sb("red", [128, NS, 32], F32, ph)
            for which, dst in ((0, sinT), (1, cosT)):
                src = ang
                if which == 1:
                    s.op("dve", "tensor_scalar", out=red[:], in0=ang[:], scalar1=math.pi / 2, scalar2=None, op0=ALU.add)
                    src = red
                s.op("dve", "tensor_scalar", out=kq[:], in0=src[:], scalar1=1.0 / TWO_PI, scalar2=None, op0=ALU.mult)
                s.op("dve", "tensor_copy", out=ki[:], in_=kq[:])
                s.op("dve", "tensor_copy", out=kq[:], in_=ki[:])
                s.op("dve", "scalar_tensor_tensor", out=red[:], in0=kq[:], scalar=-C1, in1=src[:], op0=ALU.mult, op1=ALU.add)
                s.op("dve", "scalar_tensor_tensor", out=red[:], in0=kq[:], scalar=-C2, in1=red[:], op0=ALU.mult, op1=ALU.add)
                s.op("dve", "tensor_scalar", out=red[:], in0=red[:], scalar1=math.pi, scalar2=-math.pi, op0=ALU.min, op1=ALU.max)
                s.op("act", "activation", out=dst[:], in_=red[:], func=AF.Sin)
        s.barrier()

        def rstd_act(dst, ss, inv_n, post_scale=None):
            s.op("act", "activation", out=dst, in_=ss, func=AF.Ln, scale=inv_n, bias=epsT[:, 0:1])
            if post_scale is None:
                s.op("act", "activation", out=dst, in_=dst, func=AF.Exp, scale=-0.5)
            else:
                s.op("act", "activation", out=dst, in_=dst, func=AF.Exp, scale=-0.5, bias=lnps[:, 0:1])

        def rstd_from_ss(dst, ss, n_cols, inv_n, post_scale=None):
            s.op("pool", "tensor_scalar", out=dst, in0=ss, scalar1=inv_n, scalar2=EPS, op0=ALU.mult, op1=ALU.add)
            s.op("pool", "tensor_tensor", out=dst, in0=dst, in1=mhalf[:, 0:n_cols], op=ALU.pow)
            if post_scale is not None:
                s.op("pool", "tensor_scalar", out=dst, in0=dst, scalar1=post_scale, scalar2=None, op0=ALU.mult)

        def sumsq(dst_col, src, junk):
            s.op("dve", "scalar_tensor_tensor", out=junk, in0=src, scalar=1.0, in1=src,
                 op0=ALU.mult, op1=ALU.mult, accum_out=dst_col)

        def rope(dst1, dst2, x1, x2, cs, sn, shape, t1, t2):
            s.op("dve", "tensor_tensor", out=t1, in0=x1, in1=cs, op=ALU.mult)
            s.op("dve", "tensor_tensor", out=t2, in0=x2, in1=sn, op=ALU.mult)
            s.op("dve", "tensor_tensor", out=dst1, in0=t1, in1=t2, op=ALU.subtract)
            s.op("dve", "tensor_tensor", out=t1, in0=x2, in1=cs, op=ALU.mult)
            s.op("dve", "tensor_tensor", out=t2, in0=x1, in1=sn, op=ALU.mult)
            s.op("dve", "tensor_tensor", out=dst2, in0=t1, in1=t2, op=ALU.add)

        BT = s.sb("BT", [128, 3, 16, 128], F32, phA)
        esink = s.sb("esink", [128, 16], F32, phA)
        phT5 = ExitStack()
        tblb = s.sb("tblb", [128, 32, 16], F32, phT5)
        dtab = s.sb("dtab", [128, 32, 16], F32, phT5)
        pri = s.sb("pri", [128, 128], I32, phT5)
        prf = s.sb("prf", [128, 128], F32, phT5)
        dist = s.sb("dist", [128, 128], F32, phT5)
        ge = s.sb("ge", [128, 128], F32, phT5)
        tmpb = s.sb("tmpb", [128, 16, 128], F32, phT5)
        vm = s.sb("vm", [128, 128], F32, phT5)

        def t5_gen():
            bcast_load(tblb[:].rearrange("p a b -> p (a b)"), relt, 512)
            s.op("pool", "tensor_tensor", out=dtab[:, 1:32, :], in0=tblb[:, 1:32, :], in1=tblb[:, 0:31, :],
                 op=ALU.subtract)
            s.dma("sp", pri[:], pos_r[2:3, :].to_broadcast([128, 128]))
            s.op("pool", "tensor_copy", out=prf[:], in_=pri[:])
            yield
            for ty in (0, 1):
                if ty == 0:
                    s.op("pool", "tensor_scalar", out=dist[:], in0=prf[:], scalar1=posf[:, 2:3], scalar2=0.0,
                         op0=ALU.subtract, op1=ALU.max)
                else:
                    s.op("pool", "tensor_scalar", out=dist[:], in0=prf[:], scalar1=posf[:, 1:2], scalar2=flg[:, 1:2],
                         op0=ALU.subtract, op1=ALU.mult)
                    s.op("pool", "tensor_scalar", out=ge[:], in0=prf[:], scalar1=posf[:, 3:4], scalar2=flg[:, 0:1],
                         op0=ALU.subtract, op1=ALU.mult)
                    s.op("pool", "tensor_tensor", out=dist[:], in0=dist[:], in1=ge[:], op=ALU.add)
                    s.op("pool", "tensor_scalar", out=dist[:], in0=dist[:], scalar1=0.0, scalar2=None, op0=ALU.max)
                s.op("pool", "tensor_copy", out=BT[:, ty, :, :],
                     in_=tblb[:, 0, :].unsqueeze(2).to_broadcast([128, 16, 128]))
                yield
                for b in range(1, 32):
                    s.op("pool", "tensor_single_scalar", out=ge[:], in_=dist[:], scalar=T5_THR[b - 1], op=ALU.is_ge)
                    s.op("pool", "tensor_tensor", out=tmpb[:], in0=ge[:].unsqueeze(1).to_broadcast([128, 16, 128]),
                         in1=dtab[:, b, :].unsqueeze(2).to_broadcast([128, 16, 128]), op=ALU.mult)
                    s.op("pool", "tensor_tensor", out=BT[:, ty, :, :], in0=BT[:, ty, :, :], in1=tmpb[:], op=ALU.add)
                    yield
            s.op("pool", "tensor_scalar", out=vm[:], in0=trif[:], scalar1=-1.0, scalar2=-NEG, op0=ALU.add, op1=ALU.mult)
            s.op("pool", "tensor_tensor", out=BT[:, 0, :, :], in0=BT[:, 0, :, :],
                 in1=vm[:].unsqueeze(1).to_broadcast([128, 16, 128]), op=ALU.add)
            for ty, fcol in ((2, flg[:, 0:1]), (1, flg[:, 1:2])):
                s.op("pool", "tensor_scalar", out=vm[:], in0=trif[:], scalar1=-1.0, scalar2=-1.0,
                     op0=ALU.mult, op1=ALU.subtract)
                s.op("pool", "tensor_scalar", out=vm[:], in0=vm[:], scalar1=fcol, scalar2=-1.0, op0=ALU.mult, op1=ALU.add)
                s.op("pool", "tensor_scalar", out=vm[:], in0=vm[:], scalar1=-NEG, scalar2=None, op0=ALU.mult)
                s.op("pool", "tensor_tensor", out=BT[:, ty, :, :], in0=BT[:, 1, :, :],
                     in1=vm[:].unsqueeze(1).to_broadcast([128, 16, 128]), op=ALU.add)
            yield

        t5 = t5_gen()

        def t5_step(n):
            for _ in range(n):
                try:
                    next(t5)
                except StopIteration:
                    return

        def interleave(gens, depth):
            it = iter(gens)
            active = []
            done = False
            while True:
                while not done and len(active) < depth:
                    g_ = next(it, None)
                    if g_ is None:
                        done = True
                        break
                    active.append(g_)
                if not active:
                    break
                for g_ in list(active):
                    try:
                        next(g_)
                    except StopIteration:
                        active.remove(g_)

        with ExitStack() as ph:
            w_kv = s.sb("w_kv", [128, 16, 576], BF16, ph)
            wv = w_in.rearrange("(k p) c -> p k c", p=128)
            for k0 in range(0, 16, 4):
                s.dma("pool", w_kv[:, k0:k0 + 4, 0:320], wv[:, k0:k0 + 4, 512:832])
                s.dma("pool", w_kv[:, k0:k0 + 4, 320:576], wv[:, k0:k0 + 4, 1856:2112])
            w_kvb_sb = s.sb("w_kvb_sb", [128, 2, 2048], BF16, ph)
            s.dma("pool", w_kvb_sb[:], w_kvb.rearrange("(k p) c -> p k c", p=128))
            g1b = s.sb("g1b", [128, D], F32, ph)
            bcast_load(g1b[:], g1, D)
            kvagb = s.sb("kvagb", [128, 256], F32, ph)
            bcast_load(kvagb[:], kvag, 256)
            gkpe = s.sb("gkpe", [128, 64], F32, ph)
            bcast_load(gkpe[:], mkg[128:192], 64)
            junk = s.sb("junk", [128, D], BF16, ph)

            def two(name, shape, dt):
                return [s.sb("%s_%d" % (name, i), shape, dt, ph) for i in range(2)]

            xt_ = two("xt", [128, D], F32)
            n1b_ = two("n1b", [128, D], BF16)
            n1T_ = two("n1T", [128, 16, 128], BF16)
            ss1_ = two("ss1", [128, 4], F32)
            r1_ = two("r1", [128, 4], F32)
            pab_ = two("pab", [128, 576], F32)
            kvn_ = two("kvn", [128, 256], BF16)
            kvnT_ = two("kvnT", [128, 2, 128], BF16)
            kvf_ = two("kvf", [128, 8, 256], F32)
            sq_ = two("sq", [128, 8, 128], F32)
            ssk_ = two("ssk", [128, 8], F32)
            rk_ = two("rk", [128, 8], F32)
            kpg_ = two("kpg", [128, 64], F32)
            kpr_ = two("kpr", [128, 64], F32)
            tA_ = two("tA", [128, 32], F32)
            tB_ = two("tB", [128, 32], F32)
            kn_b_ = two("kn_b", [128, 8, 128], BF16)
            kp_b_ = two("kp_b", [128, 8, 64], BF16)
            ktile = two("ktile", [128, 8, 128], BF16)
            kptile = two("kptile", [128, 4, 128], BF16)
            vtile = two("vtile", [128, 8, 129], BF16)
            ksb_ = two("ksb", [128, 128], BF16)
            ptrA = s.ps("ptrA", [128, 2048], BF16, ph)
            ptrB = s.ps("ptrB", [128, 2048], BF16, ph)
            pA1 = s.ps("pA1", [128, 512], F32, ph)
            pB1 = s.ps("pB1", [128, 512], F32, ph)
            pkv = s.ps("pkv", [128, 1024], F32, ph)
            for i in range(2):
                s.op("dve", "memset", ap=vtile[i][:], constant=1.0)

            t5_step(1000)

            def a1_tile(sl):
                par = sl % 2
                xb, n1b, n1T, ss1, r1, pab = xt_[par], n1b_[par], n1T_[par], ss1_[par], r1_[par], pab_[par]
                kvn, kvnT, kvf, sq, ssk, rk = kvn_[par], kvnT_[par], kvf_[par], sq_[par], ssk_[par], rk_[par]
                kpg, kpr, tA, tB, kn_b, kp_b, ksb = kpg_[par], kpr_[par], tA_[par], tB_[par], kn_b_[par], kp_b_[par], ksb_[par]
                s.dma("sp", xb[:], x_s[sl])
                sumsq(ss1[:, 0:1], xb[:], junk[:])
                rstd_act(r1[:, 0:1], ss1[:, 0:1], 1.0 / D)
                s.op("dve", "scalar_tensor_tensor", out=n1b[:], in0=xb[:], scalar=r1[:, 0:1], in1=g1b[:],
                     op0=ALU.mult, op1=ALU.mult)
                yield
                for kc in range(16):
                    s.op("pe", "transpose", out=ptrA[:, kc * 128:(kc + 1) * 128], in_=n1b[:, kc * 128:(kc + 1) * 128],
                         identity=ident[:])
                s.op("act", "copy", out=n1T[:].rearrange("p a b -> p (a b)"), in_=ptrA[:])
                yield
                for kc in range(16):
                    s.op("pe", "matmul", out=pA1[:, 0:320], lhsT=n1T[:, kc, :], rhs=w_kv[:, kc, 0:320],
                         start=(kc == 0), stop=(kc == 15))
                for kc in range(16):
                    s.op("pe", "matmul", out=pB1[:, 0:256], lhsT=n1T[:, kc, :], rhs=w_kv[:, kc, 320:576],
                         start=(kc == 0), stop=(kc == 15))
                s.op("act", "copy", out=pab[:, 0:320], in_=pA1[:, 0:320])
                s.op("act", "copy", out=pab[:, 320:576], in_=pB1[:, 0:256])
                yield
                sumsq(ss1[:, 1:2], pab[:, 0:256], junk[:, 0:256])
                rstd_act(r1[:, 1:2], ss1[:, 1:2], 1.0 / 256)
                s.op("dve", "scalar_tensor_tensor", out=kvn[:], in0=pab[:, 0:256], scalar=r1[:, 1:2], in1=kvagb[:],
                     op0=ALU.mult, op1=ALU.mult)
                ks3 = pab[:, 320:448].rearrange("p (a b) -> p a b", a=2)
                s.op("dve", "tensor_tensor", out=sq[:, 0:2, 0:64], in0=ks3, in1=ks3, op=ALU.mult)
                s.op("dve", "tensor_reduce", out=ss1[:, 2:4], in_=sq[:, 0:2, 0:64], axis=AX.X, op=ALU.add)
                rstd_act(r1[:, 2:4], ss1[:, 2:4], 1.0 / 64)
                s.op("dve", "tensor_tensor", out=ksb[:].rearrange("p (a b) -> p a b", a=2), in0=ks3,
                     in1=r1[:, 2:4].unsqueeze(2).to_broadcast([128, 2, 64]), op=ALU.mult)
                s.op("act", "copy", out=swa_v[:, sl, :, 0:64],
                     in_=pab[:, 448:576].rearrange("p (a b) -> p a b", a=2))
                yield
                for kc in range(2):
                    s.op("pe", "transpose", out=ptrB[:, kc * 128:(kc + 1) * 128], in_=kvn[:, kc * 128:(kc + 1) * 128],
                         identity=ident[:])
                s.op("pe", "transpose", out=ptrB[:, 256:384], in_=ksb[:], identity=ident[:])
                s.op("act", "copy", out=kvnT[:].rearrange("p a b -> p (a b)"), in_=ptrB[:, 0:256])
                s.op("act", "copy", out=swa_kt[:, sl * 128:(sl + 1) * 128], in_=ptrB[:, 256:384])
                yield
                kvf_flat = kvf[:].rearrange("p a b -> p (a b)")
                for hf in range(2):
                    for cg in range(2):
                        for kc in range(2):
                            c0_ = hf * 1024 + cg * 512
                            s.op("pe", "matmul", out=pkv[:, cg * 512:(cg + 1) * 512], lhsT=kvnT[:, kc, :],
                                 rhs=w_kvb_sb[:, kc, c0_:c0_ + 512], start=(kc == 0), stop=(kc == 1))
                    if hf == 0:
                        s.op("act", "copy", out=kvf_flat[:, 0:1024], in_=pkv[:])
                    else:
                        s.op("dve", "tensor_copy", out=kvf_flat[:, 1024:2048], in_=pkv[:])
                    yield
                s.op("dve", "tensor_tensor", out=sq[:], in0=kvf[:, :, 0:128], in1=kvf[:, :, 0:128], op=ALU.mult)
                s.op("dve", "tensor_reduce", out=ssk[:], in_=sq[:], axis=AX.X, op=ALU.add)
                sumsq(ss1[:, 2:3], pab[:, 256:320], junk[:, 0:64])
                s.op("dve", "tensor_scalar", out=ssk[:], in0=ssk[:], scalar1=ss1[:, 2:3], scalar2=None, op0=ALU.add)
                rstd_act(rk[:], ssk[:], 1.0 / 192, post_scale=192.0 ** -0.5)
                vt_ = vtile[par]
                s.op("act", "copy", out=vt_[:, :, 0:128], in_=kvf[:, :, 128:256])
                s.dma("sp", v_scr[sl], vt_[:], w=[("v", sl)])
                s.op("dve", "tensor_tensor", out=kpg[:], in0=pab[:, 256:320], in1=gkpe[:], op=ALU.mult)
                rope(kpr[:, 0:32], kpr[:, 32:64], kpg[:, 0:32], kpg[:, 32:64], cosT[:, sl, :], sinT[:, sl, :],
                     None, tA[:], tB[:])
                yield
                s.op("dve", "tensor_tensor", out=kn_b[:], in0=kvf[:, :, 0:128],
                     in1=rk[:].unsqueeze(2).to_broadcast([128, 8, 128]), op=ALU.mult)
                s.op("dve", "tensor_tensor", out=kp_b[:], in0=kpr[:].unsqueeze(1).to_broadcast([128, 8, 64]),
                     in1=rk[:].unsqueeze(2).to_broadcast([128, 8, 64]), op=ALU.mult)
                yield
                for h in range(8):
                    s.op("pe", "transpose", out=ptrB[:, 512 + h * 128:512 + (h + 1) * 128], in_=kn_b[:, h, :], identity=ident[:])
                for pr in range(4):
                    s.op("pe", "transpose", out=ptrB[:, 1536 + pr * 128:1536 + (pr + 1) * 128],
                         in_=kp_b[:, 2 * pr:2 * pr + 2, :].rearrange("p a b -> p (a b)"), identity=ident[:])
                kt_ = ktile[par]
                kpt_ = kptile[par]
                s.op("act", "copy", out=kt_[:].rearrange("p a b -> p (a b)"), in_=ptrB[:, 512:1536])
                s.op("act", "copy", out=kpt_[:].rearrange("p a b -> p (a b)"), in_=ptrB[:, 1536:2048])
                s.dma("sp", kt_scr[:, sl].rearrange("h d t -> d h t"), kt_[:], w=[("kt", sl)])
                s.dma("sp", kpe_scr[:, sl].rearrange("h d t -> d h t"), kpt_[:], w=[("kpe", sl)])
                if sl == 0:
                    dbg("kvf", kvf[:], [128, 8, 256])
                    dbg("rk", rk[:], [128, 8])
                    dbg("kpr", kpr[:], [128, 64])

            interleave((a1_tile(sl) for sl in range(NS)), 2)
        t5_step(1000)
        dbg("BT", BT[:], [128, 3, 16, 128])
        s.barrier()
        phT5.close()
        if upto == "A1":
            s.emit()
            phA.close()
            return nc

        with ExitStack() as ph:
            w_q = s.sb("w_q", [128, 16, 1536], BF16, ph)
            wv = w_in.rearrange("(k p) c -> p k c", p=128)
            for k0 in range(0, 16, 4):
                s.dma("pool", w_q[:, k0:k0 + 4, 0:512], wv[:, k0:k0 + 4, 0:512])
                s.dma("pool", w_q[:, k0:k0 + 4, 512:1536], wv[:, k0:k0 + 4, 832:1856])
            w_qb_sb = s.sb("w_qb_sb", [128, 4, 1536], BF16, ph)
            s.dma("pool", w_qb_sb[:], w_qb.rearrange("(k p) c -> p k c", p=128))
            g1b = s.sb("g1b2", [128, D], F32, ph)
            bcast_load(g1b[:], g1, D)
            qagb = s.sb("qagb", [128, 512], F32, ph)
            bcast_load(qagb[:], qag, 512)
            Gq = s.sb("Gq", [128, 192], F32, ph)
            bcast_load(Gq[:], mqg, 192)
            gtmp = s.sb("gtmp", [128, 192], F32, ph)
            bcast_load(gtmp[:], mkg, 192)
            s.op("dve", "tensor_tensor", out=Gq[:, 0:128], in0=Gq[:, 0:128], in1=gtmp[:, 0:128], op=ALU.mult)
            Gs = s.sb("Gs", [128, 64], F32, ph)
            bcast_load(Gs[:], sqg, 64)
            bcast_load(gtmp[:, 0:64], skg, 64)
            s.op("dve", "scalar_tensor_tensor", out=Gs[:], in0=Gs[:], scalar=0.125, in1=gtmp[:, 0:64],
                 op0=ALU.mult, op1=ALU.mult)
            gob = s.sb("gob_s", [128, 1024], F32, ph)
            bcast_load(gob[:], gog[1024:2048], 1024)
            bcast_load(esink[:], sinks, 16)
            s.op("act", "activation", out=esink[:], in_=esink[:], func=AF.Exp)
            s.barrier()
            xt = [s.sb("xu0", [128, D], F32, ph)] * 2
            junk = s.sb("junk2", [128, D], BF16, ph)
            n1b = s.sb("n1b2", [128, D], BF16, ph)
            n1T = s.sb("n1T2", [128, 16, 128], BF16, ph)
            ss1 = s.sb("ss2", [128, 4], F32, ph)
            r1 = s.sb("r2", [128, 4], F32, ph)
            pq = s.sb("pq", [128, 1536], F32, ph)
            qln = s.sb("qln", [128, 512], BF16, ph)
            qlT = s.sb("qlT", [128, 4, 128], BF16, ph)
            qf = s.sb("qf", [128, 8, 192], F32, ph)
            sq_raw = s.sb("sq2", [128, 1536], F32, ph)
            sq = sq_raw[:].rearrange("p (a b) -> p a b", a=8)
            qsn = sq_raw[:, 0:1024].rearrange("p (a b) -> p a b", a=16)
            ssq = s.sb("ssq", [128, 16], F32, ph)
            rq = s.sb("rq", [128, 16], F32, ph)
            qn_b = s.sb("qn_b", [128, 8, 128], BF16, ph)
            qp_b = s.sb("qp_b", [128, 8, 64], BF16, ph)
            tA = s.sb("tA2", [128, 8, 32], F32, ph)
            tB = s.sb("tB2", [128, 8, 32], F32, ph)
            qtile = [s.sb("qtile%d" % i, [128, 8, 128], BF16, ph) for i in range(2)]
            qptile = [s.sb("qptile%d" % i, [128, 4, 128], BF16, ph) for i in range(2)]
            qsb = s.sb("qsb", [128, 8, 2, 64], BF16, ph)
            qsT = s.sb("qsT", [128, 8, 128], BF16, ph)
            sbias = s.sb("sbias", [128, 8, 128], F32, ph)
            pT3 = [s.sb("pT3_%d" % i, [128, 8, 128], BF16, ph) for i in range(3)]
            den = s.sb("den", [128, 8], F32, ph)
            osw = s.sb("osw", [128, 16, 64], F32, ph)
            osn = s.sb("osn", [128, 1024], BF16, ph)
            mtile = [s.sb("mtile%d" % i, [128, 8, 128], BF16, ph) for i in range(2)]
            ptr = s.ps("ptr2", [128, 2048], BF16, ph)
            pA = s.ps("pA", [128, 1536], F32, ph)
            pB = s.ps("pB", [128, 1024], F32, ph)

            ptrS = s.ps("ptrS", [128, 1024], BF16, ph)
            qsn_t = s.sb("qsn_t", [128, 16, 64], F32, ph)
            ss_m = s.sb("ss_m", [128, 2], F32, ph)
            r_m = s.sb("r_m", [128, 2], F32, ph)
            ssq_s = s.sb("ssq_s", [128, 16], F32, ph)
            rq_s = s.sb("rq_s", [128, 16], F32, ph)
            ss_s = s.sb("ss_s", [128, 2], F32, ph)
            r_s = s.sb("r_s", [128, 2], F32, ph)
            junk_s = s.sb("junk_s", [128, 1024], BF16, ph)

            def a2_front(j):
                sl = 2 * j
                xb = xt[j % 2]
                s.dma("sp", xb[:], x_s[sl])
                sumsq(ss1[:, 0:1], xb[:], junk[:])
                rstd_from_ss(r1[:, 0:1], ss1[:, 0:1], 1, 1.0 / D)
                s.op("dve", "scalar_tensor_tensor", out=n1b[:], in0=xb[:], scalar=r1[:, 0:1], in1=g1b[:],
                     op0=ALU.mult, op1=ALU.mult)
                yield
                for kc in range(16):
                    s.op("pe", "transpose", out=ptr[:, kc * 128:(kc + 1) * 128], in_=n1b[:, kc * 128:(kc + 1) * 128],
                         identity=ident[:])
                s.op("act", "copy", out=n1T[:].rearrange("p a b -> p (a b)"), in_=ptr[:])
                yield
                yield
                for kc in range(16):
                    s.op("pe", "matmul", out=pA[:, 0:512], lhsT=n1T[:, kc, :], rhs=w_q[:, kc, 0:512],
                         start=(kc == 0), stop=(kc == 15))
                s.op("act", "copy", out=pq[:, 0:512], in_=pA[:, 0:512])
                yield
                for cg in range(2):
                    for kc in range(16):
                        s.op("pe", "matmul", out=pB[:, cg * 512:(cg + 1) * 512], lhsT=n1T[:, kc, :],
                             rhs=w_q[:, kc, 512 + cg * 512:512 + (cg + 1) * 512], start=(kc == 0), stop=(kc == 15))
                s.op("act", "copy", out=pq[:, 512:1536], in_=pB[:])

            def a2_mla(j):
                sl = 2 * j
                sumsq(ss_m[:, 0:1], pq[:, 0:512], junk[:, 0:512])
                rstd_from_ss(r_m[:, 0:1], ss_m[:, 0:1], 1, 1.0 / 512)
                s.op("dve", "scalar_tensor_tensor", out=qln[:], in0=pq[:, 0:512], scalar=r_m[:, 0:1], in1=qagb[:],
                     op0=ALU.mult, op1=ALU.mult)
                yield
                for kc in range(4):
                    s.op("pe", "transpose", out=ptr[:, kc * 128:(kc + 1) * 128], in_=qln[:, kc * 128:(kc + 1) * 128],
                         identity=ident[:])
                s.op("act", "copy", out=qlT[:].rearrange("p a b -> p (a b)"), in_=ptr[:, 0:512])
                yield
                for cg in range(3):
                    for kc in range(4):
                        s.op("pe", "matmul", out=pA[:, cg * 512:(cg + 1) * 512], lhsT=qlT[:, kc, :],
                             rhs=w_qb_sb[:, kc, cg * 512:(cg + 1) * 512], start=(kc == 0), stop=(kc == 3))
                s.op("act", "copy", out=qf[:].rearrange("p a b -> p (a b)"), in_=pA[:])
                yield
                s.op("dve", "tensor_tensor", out=sq[:], in0=qf[:], in1=qf[:], op=ALU.mult)
                s.op("dve", "tensor_reduce", out=ssq[:, 0:8], in_=sq[:], axis=AX.X, op=ALU.add)
                rstd_from_ss(rq[:, 0:8], ssq[:, 0:8], 8, 1.0 / 192)
                yield
                s.op("dve", "tensor_tensor", out=qf[:], in0=qf[:], in1=rq[:, 0:8].unsqueeze(2).to_broadcast([128, 8, 192]),
                     op=ALU.mult)
                s.op("dve", "tensor_tensor", out=qf[:], in0=qf[:], in1=Gq[:].unsqueeze(1).to_broadcast([128, 8, 192]),
                     op=ALU.mult)
                s.op("pool", "tensor_copy", out=qn_b[:], in_=qf[:, :, 0:128])
                csb = cosT[:, sl, :].unsqueeze(1).to_broadcast([128, 8, 32])
                snb = sinT[:, sl, :].unsqueeze(1).to_broadcast([128, 8, 32])
                rope(qp_b[:, :, 0:32], qp_b[:, :, 32:64], qf[:, :, 128:160], qf[:, :, 160:192], csb, snb, None,
                     tA[:], tB[:])
                yield
                for h in range(8):
                    s.op("pe", "transpose", out=ptr[:, h * 128:(h + 1) * 128], in_=qn_b[:, h, :], identity=ident[:])
                for pr in range(4):
                    s.op("pe", "transpose", out=ptr[:, 1024 + pr * 128:1024 + (pr + 1) * 128],
                         in_=qp_b[:, 2 * pr:2 * pr + 2, :].rearrange("p a b -> p (a b)"), identity=ident[:])
                qt_ = qtile[j % 2]
                qpt_ = qptile[j % 2]
                s.op("act", "copy", out=qt_[:].rearrange("p a b -> p (a b)"), in_=ptr[:, 0:1024])
                s.op("act", "copy", out=qpt_[:].rearrange("p a b -> p (a b)"), in_=ptr[:, 1024:1536])
                s.dma("sp", qt_scr[:, j].rearrange("h d t -> d h t"), qt_[:], w=[("qt", j)])
                s.dma("sp", qpe_scr[:, j].rearrange("h d t -> d h t"), qpt_[:], w=[("qpe", j)])

            def a2_swa(j):
                qs3 = pq[:, 512:1536].rearrange("p (a b) -> p a b", a=16)
                s.op("dve", "tensor_tensor", out=qsn_t[:], in0=qs3, in1=qs3, op=ALU.mult)
                s.op("dve", "tensor_reduce", out=ssq_s[:], in_=qsn_t[:], axis=AX.X, op=ALU.add)
                rstd_from_ss(rq_s[:], ssq_s[:], 16, 1.0 / 64)
                yield
                s.op("dve", "tensor_tensor", out=qsn_t[:], in0=qs3, in1=rq_s[:].unsqueeze(2).to_broadcast([128, 16, 64]),
                     op=ALU.mult)
                s.op("dve", "tensor_tensor", out=qsb[:].rearrange("p i g d -> p g i d"),
                     in0=qsn_t[:].rearrange("p (g i) d -> p g i d", g=2),
                     in1=Gs[:].unsqueeze(1).unsqueeze(1).to_broadcast([128, 2, 8, 64]), op=ALU.mult)
                yield
                for i in range(8):
                    s.op("pe", "transpose", out=ptrS[:, i * 128:(i + 1) * 128],
                         in_=qsb[:, i, :, :].rearrange("p a b -> p (a b)"), identity=ident[:])
                s.op("act", "copy", out=qsT[:].rearrange("p a b -> p (a b)"), in_=ptrS[:])
                yield
                types = [(0, 2 * j), (2, 2 * j + 1)] + ([(1, 2 * j - 1)] if j >= 1 else [])
                pO = pA[:, 0:1024].rearrange("p (a b) -> p a b", a=8)
                for g in range(2):
                    for ti, (ty, kslot) in enumerate(types):
                        for half in range(2):
                            s.op("pe", "matmul", out=pB[:, half * 512:(half + 1) * 512],
                                 lhsT=swa_kt[64 * g:64 * g + 64, kslot * 128:(kslot + 1) * 128],
                                 rhs=qsT[64 * g:64 * g + 64, half * 4:half * 4 + 4, :].rearrange("p a b -> p (a b)"),
                                 start=True, stop=True)
                        s.op("dve", "tensor_tensor", out=sbias[:].rearrange("p a b -> p (a b)"), in0=pB[:],
                             in1=BT[:, ty, g * 8:(g + 1) * 8, :].rearrange("p a b -> p (a b)"), op=ALU.add)
                        s.op("act", "activation", out=pT3[ti][:], in_=sbias[:], func=AF.Exp)
                        yield
                    for i in range(8):
                        for ti, (ty, kslot) in enumerate(types):
                            s.op("pe", "matmul", out=pO[:, i, 0:65], lhsT=pT3[ti][:, i, :], rhs=swa_v[:, kslot, g, :],
                                 start=(ti == 0), stop=(ti == len(types) - 1))
                    s.op("dve", "tensor_tensor", out=den[:], in0=pO[:, :, 64], in1=esink[:, g * 8:(g + 1) * 8], op=ALU.add)
                    s.op("dve", "reciprocal", out=den[:], in_=den[:])
                    s.op("dve", "tensor_tensor", out=osw[:, g * 8:(g + 1) * 8, :], in0=pO[:, :, 0:64],
                         in1=den[:].unsqueeze(2).to_broadcast([128, 8, 64]), op=ALU.mult)
                    yield
                osw_f = osw[:].rearrange("p a b -> p (a b)")
                if j == 1:
                    dbg("osw", osw_f, [128, 1024])
                sumsq(ss_s[:, 0:1], osw_f, junk_s[:])
                rstd_from_ss(r_s[:, 0:1], ss_s[:, 0:1], 1, 1.0 / 1024)
                s.op("dve", "scalar_tensor_tensor", out=osn[:], in0=osw_f, scalar=r_s[:, 0:1], in1=gob[:],
                     op0=ALU.mult, op1=ALU.mult)
                yield
                for i in range(8):
                    s.op("pe", "transpose", out=ptrS[:, i * 128:(i + 1) * 128], in_=osn[:, i * 128:(i + 1) * 128],
                         identity=ident[:])
                mt_ = mtile[j % 2]
                s.op("act", "copy", out=mt_[:].rearrange("p a b -> p (a b)"), in_=ptrS[:])
                s.dma("sp", mix_scr[j], mt_[:], w=[("mix", j)])

            for _ in a2_front(0):
                pass
            for j in range(NT):
                gens = [a2_mla(j), a2_swa(j)]
                if j + 1 < NT:
                    gens.append(a2_front(j + 1))
                interleave(iter(gens), 3)
        s.barrier()
        phA.close()
        if upto == "A2":
            s.emit()
            return nc

        with ExitStack() as phB:
            o_all = s.sb("o_all", [128, NT, 8, 128], F32, phB)
            with ExitStack() as ph:
                KT = [s.sb("KT%d" % i, [128, NS, 128], BF16, ph) for i in range(2)]
                VV = [s.sb("VV%d" % i, [128, NS, 129], BF16, ph) for i in range(2)]
                QT = [s.sb("QT%d" % i, [128, NT, 128], BF16, ph) for i in range(2)]
                KP = [s.sb("KP%d" % i, [128, NS, 128], BF16, ph) for i in range(2)]
                QP = [s.sb("QP%d" % i, [128, NT, 128], BF16, ph) for i in range(2)]
                pT = [s.sb("pT%d" % i, [128, 4, 128], BF16, ph) for i in range(6)]
                rec = s.sb("rec", [128, 1], F32, ph)
                pS = [s.ps("pS%d" % i, [128, 512], F32, ph) for i in range(5)]
                pO = [s.ps("pO%d" % i, [128, 512], F32, ph) for i in range(3)]
                cstate = [0]
                for h in range(8):
                    b_ = h % 2
                    hb = (h % 2) * 64
                    s.dma("sp", KT[b_][:], kt_scr[h].rearrange("s d t -> d s t"), r=[("kt", i) for i in range(NS)])
                    s.dma("sp", VV[b_][:], v_scr[:, :, h, :].rearrange("s k c -> k s c"), r=[("v", i) for i in range(NS)])
                    s.dma("sp", QT[b_][:], qt_scr[h].rearrange("s d t -> d s t"), r=[("qt", i) for i in range(NT)])
                    s.dma("sp", KP[b_][:], kpe_scr[h // 2].rearrange("s d t -> d s t"), r=[("kpe", i) for i in range(NS)])
                    s.dma("sp", QP[b_][:], qpe_scr[h // 2].rearrange("s d t -> d s t"), r=[("qpe", i) for i in range(NT)])
                    chunks = []
                    for j in range(NT):
                        nsl = 2 * j + 2
                        for c0 in range(0, nsl, 4):
                            chunks.append((j, c0, min(4, nsl - c0), nsl))

                    def emit_scores(ch):
                        j, c0, cn, nsl = ch
                        nonlocal_c = cstate
                        ps_ = pS[nonlocal_c[0] % 5]
                        pt_ = pT[nonlocal_c[0] % 6]
                        nonlocal_c[0] += 1
                        for si in range(cn):
                            sl = c0 + si
                            s.op("pe", "matmul", out=ps_[:, si * 128:(si + 1) * 128], lhsT=KT[b_][:, sl, :],
                                 rhs=QT[b_][:, j, :], start=True, stop=False)
                            s.op("pe", "matmul", out=ps_[:, si * 128:(si + 1) * 128], lhsT=KP[b_][hb:hb + 64, sl, :],
                                 rhs=QP[b_][hb:hb + 64, j, :], start=False, stop=True)
                        s.op("act", "activation", out=pt_[:, 0:cn, :].rearrange("p a b -> p (a b)"),
                             in_=ps_[:, 0:cn * 128], func=AF.Exp)
                        for si in range(cn):
                            sl = c0 + si
                            if sl == 2 * j:
                                s.op("pool", "tensor_tensor", out=pt_[:, si, :], in0=pt_[:, si, :], in1=tri[:], op=ALU.mult)
                            elif sl == 2 * j + 1:
                                s.op("pool", "tensor_scalar", out=pt_[:, si, :], in0=pt_[:, si, :], scalar1=flg[:, 0:1],
                                     scalar2=None, op0=ALU.mult)
                        return pt_

                    def emit_pv(ch, pt_):
                        j, c0, cn, nsl = ch
                        po = pO[j % 3]
                        for si in range(cn):
                            sl = c0 + si
                            s.op("pe", "matmul", out=po[:, 0:129], lhsT=pt_[:, si, :], rhs=VV[b_][:, sl, :],
                                 start=(sl == 0), stop=(sl == nsl - 1))
                        if c0 + cn == nsl:
                            s.op("dve", "reciprocal", out=rec[:], in_=po[:, 128:129])
                            s.op("dve", "tensor_scalar", out=o_all[:, j, h, :], in0=po[:, 0:128], scalar1=rec[:, 0:1],
                                 scalar2=None, op0=ALU.mult)

                    LOOK = 3
                    pend = []
                    for ci_, ch in enumerate(chunks):
                        pend.append((ch, emit_scores(ch)))
                        if len(pend) > LOOK:
                            c_, p_ = pend.pop(0)
                            emit_pv(c_, p_)
                    while pend:
                        c_, p_ = pend.pop(0)
                        emit_pv(c_, p_)
            s.barrier()
            dbg("o_all1", o_all[:, 1, :, :], [128, 8, 128])
            with ExitStack() as ph:
                w_o = s.sb("w_o", [128, 16, D], BF16, ph)
                wv = w_out.rearrange("(k p) c -> p k c", p=128)
                for k0 in range(0, 16, 2):
                    s.dma("pool", w_o[:, k0:k0 + 2, :], wv[:, k0:k0 + 2, :])
                gob = s.sb("gob_m", [128, 1024], F32, ph)
                bcast_load(gob[:], gog[0:1024], 1024)
                g2b = s.sb("g2b", [128, D], F32, ph)
                bcast_load(g2b[:], g2, D)
                xt = [s.sb("xv%d" % i, [128, D], F32, ph) for i in range(2)]
                junk = s.sb("junk3", [128, D], BF16, ph)
                ss3_ = [s.sb("ss3_%d" % i, [128, 2], F32, ph) for i in range(2)]
                r3_ = [s.sb("r3_%d" % i, [128, 2], F32, ph) for i in range(2)]
                omn_ = [s.sb("omn%d" % i, [128, 1024], BF16, ph) for i in range(2)]
                mixT = [s.sb("mixT%d" % i, [128, 16, 128], BF16, ph) for i in range(2)]
                hf = [s.sb("hf%d" % i, [128, D], F32, ph) for i in range(2)]
                hnb_ = [s.sb("hnb%d" % i, [128, D], BF16, ph) for i in range(2)]
                hnT = [s.sb("hnT%d" % i, [128, 16, 128], BF16, ph) for i in range(2)]
                ptr = s.ps("ptr3", [128, 1024], BF16, ph)
                ptr4 = s.ps("ptr4", [128, 2048], BF16, ph)
                pH = s.ps("pH", [128, 2048], F32, ph)

                def b2_tile(j):
                    par = j % 2
                    ss1, r1, omn, hnb = ss3_[par], r3_[par], omn_[par], hnb_[par]
                    xb = xt[par]
                    s.dma("sp", xb[:], x_s[2 * j])
                    mx = mixT[par]
                    s.dma("sp", mx[:, 8:16, :], mix_scr[j], r=[("mix", j)])
                    om = o_all[:, j, :, :].rearrange("p a b -> p (a b)")
                    sumsq(ss1[:, 0:1], om, junk[:, 0:1024])
                    rstd_from_ss(r1[:, 0:1], ss1[:, 0:1], 1, 1.0 / 1024)
                    s.op("dve", "scalar_tensor_tensor", out=omn[:], in0=om, scalar=r1[:, 0:1], in1=gob[:],
                         op0=ALU.mult, op1=ALU.mult)
                    yield
                    for i in range(8):
                        s.op("pe", "transpose", out=ptr[:, i * 128:(i + 1) * 128], in_=omn[:, i * 128:(i + 1) * 128],
                             identity=ident[:])
                    s.op("act", "copy", out=mx[:, 0:8, :].rearrange("p a b -> p (a b)"), in_=ptr[:])
                    yield
                    for cg in range(4):
                        for kc in range(16):
                            s.op("pe", "matmul", out=pH[:, cg * 512:(cg + 1) * 512], lhsT=mx[:, kc, :],
                                 rhs=w_o[:, kc, cg * 512:(cg + 1) * 512], start=(kc == 0), stop=(kc == 15))
                    hb_ = hf[par]
                    s.op("dve", "tensor_tensor", out=hb_[:], in0=pH[:], in1=xb[:], op=ALU.add)
                    s.dma("sp", out_h[j], hb_[:], w=[("h", j)])
                    yield
                    sumsq(ss1[:, 1:2], hb_[:], junk[:])
                    rstd_from_ss(r1[:, 1:2], ss1[:, 1:2], 1, 1.0 / D)
                    s.op("dve", "scalar_tensor_tensor", out=hnb[:], in0=hb_[:], scalar=r1[:, 1:2], in1=g2b[:],
                         op0=ALU.mult, op1=ALU.mult)
                    yield
                    for kc in range(16):
                        s.op("pe", "transpose", out=ptr4[:, kc * 128:(kc + 1) * 128], in_=hnb[:, kc * 128:(kc + 1) * 128],
                             identity=ident[:])
                    ht_ = hnT[par]
                    s.op("act", "copy", out=ht_[:].rearrange("p a b -> p (a b)"), in_=ptr4[:])
                    s.dma("sp", hn_scr[j // 4][:, :, j % 4, :], ht_[:], w=[("hn", j)])

                interleave((b2_tile(j) for j in range(NT)), 2)
        s.barrier()
        if upto == "B":
            s.emit()
            return nc

        with ExitStack() as ph:
            w_pq = s.sb("w_pq", [128, 16, D], BF16, ph)
            wv = pwq.rearrange("(k p) c -> p k c", p=128)
            for k0 in range(0, 16, 2):
                s.dma("pool", w_pq[:, k0:k0 + 2, :], wv[:, k0:k0 + 2, :])
            hg = [s.sb("hg%d" % i, [128, 16, 4, 128], BF16, ph) for i in range(2)]
            qg = [s.sb("qg%d" % i, [128, 16, 512], BF16, ph) for i in range(2)]
            pQ = [s.ps("pQ%d" % i, [128, 512], F32, ph) for i in range(2)]
            for g in range(NGRP):
                hg_ = hg[g % 2]
                qg_ = qg[g % 2]
                s.dma("sp", hg_[:], hn_scr[g], r=[("hn", 4 * g + i) for i in range(4)])
                for c in range(16):
                    pq_ = pQ[c % 2]
                    for kc in range(16):
                        s.op("pe", "matmul", out=pq_[:], lhsT=w_pq[:, kc, c * 128:(c + 1) * 128],
                             rhs=hg_[:, kc, :, :].rearrange("p a b -> p (a b)"), start=(kc == 0), stop=(kc == 15))
                    if c % 2 == 0:
                        s.op("act", "copy", out=qg_[:, c, :], in_=pq_[:])
                    else:
                        s.op("dve", "tensor_copy", out=qg_[:, c, :], in_=pq_[:])
                s.dma("sp", q_scr[g], qg_[:], w=[("q", g)])
        s.barrier()

        if upto == "C1":
            s.emit()
            return nc

        with ExitStack() as ph:
            skT = s.sb("skT_sb", [128, 16, 128], BF16, ph)
            s.dma("pool", skT[:], skT_d)
            hg = s.sb("hgc", [128, 16, 4, 128], BF16, ph)
            NGB = 4
            exgt = s.sb("exgt", [128, 2 * NGB, 2048], BF16, ph)
            qg = exgt[:, 0:4, :].rearrange("p a (b c) -> p (a b) c", c=512)
            EXGT_KEYS = [("ex", b, rr) for b in range(NGB) for rr in range(16)]
            S0t = s.sb("S0t", [128, 32, 128], F32, ph)
            S1a = s.sb("S1a", [128, 32, 128], F32, ph)
            T01 = s.sb("T01", [128, 2, 8, 16], F32, ph)
            wrk = s.sb("wrk", [128, 256], F32, ph)
            Ct = s.sb("Ct", [128, 8, 16], F32, ph)
            Ce = s.sb("Ce", [128, 8, 16], F32, ph)
            Zs = s.sb("Zs", [128, 8], F32, ph)
            en = s.sb("en", [128, 8], F32, ph)
            tau = s.sb("tau", [128, 8], F32, ph)
            Dg = s.sb("Dg", [128, 32, 128], BF16, ph)
            yacc = s.sb("yacc", [128, 4, D], F32, ph)
            cand_t = s.sb("cand", [128, 8, 256], F32, ph)
            cand = cand_t[:]
            dd1 = cand_t[:].rearrange("p a (c b) -> p (a c) b", c=2)
            ex = [exgt[:, i, :].rearrange("p (a b) -> p a b", a=16) for i in range(NGB)]
            Gt = [exgt[:, NGB + i, :].rearrange("p (a b) -> p a b", a=16) for i in range(NGB)]
            WTs = [s.sb("WTs%d" % i, [128, NB, 512], BF16, ph) for i in range(2)]
            gu = 0
            gel = s.sb("gel", [128, NB, 512], BF16, ph)
            WgT = s.sb("WgT", [128, NB, 512], BF16, ph)
            NU = 5
            UT = [s.sb("UT%d" % i, [128, 16, 128], BF16, ph) for i in range(NU)]
            Vc = [s.sb("Vc%d" % i, [128, D], BF16, ph) for i in range(2 * NB)]
            pAa = [s.ps("pAa%d" % i, [128, 512], F32, ph) for i in range(4)]
            pSc = pAa[0]
            pW = [s.ps("pW%d" % i, [128, 512], F32, ph) for i in range(2)]
            pY = [s.ps("pY%d" % i, [128, 512], F32, ph) for i in range(2)]
            for g in range(ngrp):
                s.dma("sp", hg[:], hn_scr[g], r=[("hn", 4 * g + i) for i in range(4)])
                s.dma("sp", qg, q_scr[g], r=[("q", g)], w=EXGT_KEYS)
                s.dma("sp", yacc[:], out_h[4 * g:4 * g + 4].rearrange("j p d -> p j d"), r=[("h", 4 * g + i) for i in range(4)])
                for tt in range(4):
                    for c0 in range(0, 16, 4):
                        for ci in range(4):
                            c = c0 + ci
                            s.op("pe", "matmul", out=pSc[:, ci * 128:(ci + 1) * 128], lhsT=qg[:, c, tt * 128:(tt + 1) * 128],
                                 rhs=skT[:, c, :], start=True, stop=True, r=EXGT_KEYS + [skT.name], w=[pSc.name])
                        for ci in range(4):
                            c = c0 + ci
                            h, half = c // 2, c % 2
                            dst = (S0t if half == 0 else S1a)[:, tt * 8 + h, :]
                            if (c0 // 4) % 2 == 0:
                                s.op("act", "copy", out=dst, in_=pSc[:, ci * 128:(ci + 1) * 128])
                            else:
                                s.op("dve", "tensor_copy", out=dst, in_=pSc[:, ci * 128:(ci + 1) * 128])
                    for half in range(2):
                        src = S0t if half == 0 else S1a
                        for h in range(8):
                            s.op("dve", "max", out=T01[:, half, h, 0:8], in_=src[:, tt * 8 + h, :])
                            s.op("dve", "match_replace", out=wrk[:, 0:128], in_to_replace=T01[:, half, h, 0:8],
                                 in_values=src[:, tt * 8 + h, :], imm_value=-1e30)
                            s.op("dve", "max", out=T01[:, half, h, 8:16], in_=wrk[:, 0:128])
                    s.op("dve", "tensor_tensor", out=cand[:].rearrange("p h (a b) -> p h a b", a=16),
                         in0=T01[:, 0, :, :].unsqueeze(3).to_broadcast([128, 8, 16, 16]),
                         in1=T01[:, 1, :, :].unsqueeze(2).to_broadcast([128, 8, 16, 16]), op=ALU.add)
                    for h in range(8):
                        s.op("dve", "max", out=Ct[:, h, 0:8], in_=cand[:, h, :])
                        s.op("dve", "match_replace", out=wrk[:], in_to_replace=Ct[:, h, 0:8], in_values=cand[:, h, :],
                             imm_value=-1e30)
                        s.op("dve", "max", out=Ct[:, h, 8:16], in_=wrk[:])
                    s.op("dve", "tensor_tensor", out=Ce[:], in0=Ct[:], in1=Ct[:, :, 0:1].to_broadcast([128, 8, 16]),
                         op=ALU.subtract)
                    s.op("act", "activation", out=Ce[:], in_=Ce[:], func=AF.Exp)
                    s.op("dve", "tensor_reduce", out=Zs[:], in_=Ce[:], axis=AX.X, op=ALU.add)
                    s.op("dve", "reciprocal", out=Zs[:], in_=Zs[:])
                    s.op("dve", "tensor_tensor", out=en[:], in0=Ce[:, :, 15], in1=Zs[:], op=ALU.mult)
                    s.op("dve", "tensor_scalar", out=tau[:], in0=Ct[:, :, 15], scalar1=-DELTA, scalar2=None, op0=ALU.add)
                    s.op("dve", "tensor_tensor", out=S0t[:, tt * 8:(tt + 1) * 8, :], in0=S0t[:, tt * 8:(tt + 1) * 8, :],
                         in1=tau[:].unsqueeze(2).to_broadcast([128, 8, 128]), op=ALU.subtract)
                    s.op("dve", "tensor_tensor", out=Dg[:, tt * 8:(tt + 1) * 8, :],
                         in0=ident[:].unsqueeze(1).to_broadcast([128, 8, 128]),
                         in1=en[:].unsqueeze(2).to_broadcast([128, 8, 128]), op=ALU.mult)
                    s.op("pool", "tensor_scalar", out=S1a[:, tt * 8:(tt + 1) * 8, :], in0=S1a[:, tt * 8:(tt + 1) * 8, :],
                         scalar1=-1.0, scalar2=None, op0=ALU.mult)
                    if g == 0 and tt == 0:
                        dbg("Ct", Ct[:], [128, 8, 16])
                        dbg("en", en[:], [128, 8])
                if upto == "C3a":
                    break
                def load_u(chunk):
                    s.dma("pool", UT[chunk % NU][:], ut_t[chunk])

                def load_v(blk_):
                    for i in range(NB):
                        n0_ = blk_ * NB + i
                        s.dma("pool", Vc[(blk_ % 2) * NB + i][:], pv[n0_ * 128:(n0_ + 1) * 128, :])

                def gate_ops(u):
                    b = u % NGB
                    n0 = u // 2
                    hh = u % 2
                    exkeys = [("ex", b, rr) for rr in range(16)]
                    if u % 4 != 3:
                        for rr in range(16):
                            row = 16 * hh + rr
                            s.op("act", "activation", out=ex[b][:, rr, :], in_=S1a[:, row, :], func=AF.Exp, scale=-1.0,
                                 bias=S0t[:, row, n0:n0 + 1], r=[S1a.name, S0t.name], w=[("ex", b, rr)])
                        s.op("dve", "tensor_tensor", out=Gt[b], in0=S1a[:, 16 * hh:16 * hh + 16, :],
                             in1=S0t[:, 16 * hh:16 * hh + 16, n0:n0 + 1].to_broadcast([128, 16, 128]), op=ALU.is_le,
                             r=[S1a.name, S0t.name], w=[("Gt", b)])
                        s.op("dve", "tensor_tensor", out=Gt[b], in0=Gt[b], in1=ex[b], op=ALU.mult,
                             r=[("Gt", b)] + exkeys, w=[("Gt", b)])
                    else:
                        s.op("pool", "tensor_tensor", out=dd1,
                             in0=S0t[:, 16 * hh:16 * hh + 16, n0:n0 + 1].to_broadcast([128, 16, 128]),
                             in1=S1a[:, 16 * hh:16 * hh + 16, :], op=ALU.subtract)
                        s.op("act", "activation", out=ex[b], in_=dd1, func=AF.Exp, r=[cand_t.name], w=exkeys)
                        s.op("dve", "scalar_tensor_tensor", out=Gt[b].rearrange("p a b -> p (a b)"),
                             in0=dd1.rearrange("p a b -> p (a b)"), scalar=0.0, in1=ex[b].rearrange("p a b -> p (a b)"),
                             op0=ALU.is_ge, op1=ALU.mult, r=[cand_t.name] + exkeys, w=[("Gt", b)])

                def gate_mm(u):
                    b = u % NGB
                    n0 = u // 2
                    hh = u % 2
                    blk_, i = n0 // NB, n0 % NB
                    pw_ = pW[n0 % 2]
                    for t2 in range(2):
                        tt = 2 * hh + t2
                        for h in range(8):
                            s.op("pe", "matmul", out=pw_[:, tt * 128:(tt + 1) * 128], lhsT=Gt[b][:, t2 * 8 + h, :],
                                 rhs=Dg[:, tt * 8 + h, :], start=(h == 0), stop=(h == 7),
                                 r=[("Gt", b), Dg.name], w=[pw_.name])
                    if hh == 1:
                        s.op("dve", "tensor_copy", out=WTs[blk_ % 2][:, i, :], in_=pw_[:])

                def a_mm(blk_, i):
                    chunk = blk_ * NB + i
                    pa_ = pAa[chunk % 4]
                    for kc in range(16):
                        s.op("pe", "matmul", out=pa_[:], lhsT=UT[chunk % NU][:, kc, :],
                             rhs=hg[:, kc, :, :].rearrange("p a b -> p (a b)"), start=(kc == 0), stop=(kc == 15))

                def a_post(blk_, i):
                    pa_ = pAa[(blk_ * NB + i) % 4]
                    s.op("act", "activation", out=gel[:, i, :], in_=pa_[:], func=AF.Gelu_apprx_tanh)

                def a_mult(blk_, i):
                    s.op("dve", "tensor_tensor", out=WgT[:, i, :], in0=WTs[blk_ % 2][:, i, :], in1=gel[:, i, :], op=ALU.mult)

                yctr = [0]

                def y_items(blk_, q):
                    tt = q
                    for dg in range(4):
                        py_ = pY[yctr[0] % 2]
                        yctr[0] += 1
                        for i in range(NB):
                            s.op("pe", "matmul", out=py_[:], lhsT=WgT[:, i, tt * 128:(tt + 1) * 128],
                                 rhs=Vc[(blk_ % 2) * NB + i][:, dg * 512:(dg + 1) * 512], start=(i == 0), stop=(i == NB - 1))
                        s.op("dve", "tensor_tensor", out=yacc[:, tt, dg * 512:(dg + 1) * 512],
                             in0=py_[:], in1=yacc[:, tt, dg * 512:(dg + 1) * 512], op=ALU.add)

                NUN = nblk * 2 * NB
                for c in range(min(NU, nblk * NB)):
                    load_u(c)
                load_v(0)
                nu_loaded = min(NU, nblk * NB)
                st = {"gops": 0, "gmm": 0}

                def pump(limit, force_mm=False):
                    if st["gmm"] < min(limit, st["gops"]) and (force_mm or st["gops"] - st["gmm"] >= min(NGB, limit - st["gmm"])):
                        gate_mm(st["gmm"])
                        st["gmm"] += 1
                    while st["gops"] < limit and st["gops"] - st["gmm"] < NGB:
                        gate_ops(st["gops"])
                        st["gops"] += 1

                lim0 = min(NUN, 2 * NB)
                while st["gmm"] < lim0:
                    pump(lim0, force_mm=True)
                for blk in range(nblk):
                    lim = min(NUN, (blk + 2) * 2 * NB)
                    if blk + 1 < nblk:
                        load_v(blk + 1)
                    for k in range(2 * NB):
                        if k < NB:
                            a_mm(blk, k)
                            if k == NB - 1:
                                for i_ in range(NB):
                                    a_post(blk, i_)
                                for i_ in range(NB):
                                    a_mult(blk, i_)
                            if nu_loaded < nblk * NB and nu_loaded - NU <= blk * NB + k:
                                load_u(nu_loaded)
                                nu_loaded += 1
                        else:
                            y_items(blk, k - NB)
                        pump(lim)
                    while st["gmm"] < lim:
                        pump(lim, force_mm=True)
                s.dma("sp", out_h[4 * g:4 * g + 4].rearrange("j p d -> p j d"), yacc[:], w=[("h", 4 * g + i) for i in range(4)])
        s.emit()
    return nc


_NC_CACHE = {}


def _layout_inputs(inp):
    x = np.asarray(inp["x"], dtype=np.float32)
    pos = np.asarray(inp["positions"], dtype=np.int32)
    half = 32
    invf = (10000.0 ** (-np.arange(half, dtype=np.float32) / half)).astype(np.float32)
    invf_t = np.ascontiguousarray(np.broadcast_to(invf, (128, 32))).astype(np.float32)
    U = np.asarray(inp["peer_u"], dtype=np.float32)[0]
    ut_t = np.ascontiguousarray(U.reshape(NCHUNK, 128, 16, 128).transpose(0, 3, 2, 1))
    sk = np.asarray(inp["peer_sub_keys"], dtype=np.float32)[0]
    skT = np.ascontiguousarray(sk.reshape(16, 128, 128).transpose(2, 0, 1))
    shared = {
        "invf": invf_t,
        "norm1_gain": np.ascontiguousarray(inp["norm1_gain"][0], dtype=np.float32),
        "w_in": np.ascontiguousarray(inp["w_in"][0], dtype=np.float32),
        "q_a_gain": np.ascontiguousarray(inp["q_a_gain"][0], dtype=np.float32),
        "w_q_b": np.ascontiguousarray(inp["w_q_b"][0], dtype=np.float32),
        "kv_a_gain": np.ascontiguousarray(inp["kv_a_gain"][0], dtype=np.float32),
        "w_kv_b": np.ascontiguousarray(inp["w_kv_b"][0], dtype=np.float32),
        "mla_q_gain": np.ascontiguousarray(inp["mla_q_gain"][0], dtype=np.float32),
        "mla_k_gain": np.ascontiguousarray(inp["mla_k_gain"][0], dtype=np.float32),
        "swa_q_gain": np.ascontiguousarray(inp["swa_q_gain"][0], dtype=np.float32),
        "swa_k_gain": np.ascontiguousarray(inp["swa_k_gain"][0], dtype=np.float32),
        "swa_sinks": np.ascontiguousarray(inp["swa_sinks"][0], dtype=np.float32),
        "rel_bias_table": np.ascontiguousarray(np.asarray(inp["rel_bias_table"], dtype=np.float32).reshape(-1)),
        "group_out_gain": np.ascontiguousarray(inp["group_out_gain"][0], dtype=np.float32),
        "w_out": np.ascontiguousarray(inp["w_out"][0], dtype=np.float32),
        "norm2_gain": np.ascontiguousarray(inp["norm2_gain"][0], dtype=np.float32),
        "peer_w_q": np.ascontiguousarray(inp["peer_w_q"][0], dtype=np.float32),
        "skT": skT,
        "ut_t": ut_t,
        "peer_v": np.ascontiguousarray(inp["peer_v"][0], dtype=np.float32),
    }
    in_maps = []
    for c in range(8):
        b, p = c // 2, c % 2
        xb = x[b].reshape(NS, 128, D)
        pb = pos[b].reshape(NS, 128)
        order = []
        for j in range(NT):
            order += [2 * j + p, 2 * j + 1 - p]
        order = np.array(order)
        m = dict(shared)
        m["x_s"] = np.ascontiguousarray(xb[order])
        ps_ = pb[order]
        m["pos_r"] = np.ascontiguousarray(ps_)
        m["pos_s"] = np.ascontiguousarray(ps_.T)
        fl = np.zeros((128, 2), np.float32)
        fl[:, 0] = 1.0 if p == 1 else 0.0
        fl[:, 1] = 1.0 if p == 0 else 0.0
        m["flags"] = fl
        in_maps.append(m)
    return in_maps


def _assemble(results, key="out"):
    out = np.zeros((4, 4096, D), np.float32)
    ov = out.reshape(4, NS, 128, D)
    for c in range(8):
        b, p = c // 2, c % 2
        r = np.asarray(results[c][key]).reshape(NT, 128, D)
        for j in range(NT):
            ov[b, 2 * j + p] = r[j]
    return out


def kernel(**inputs):
    if "nc" not in _NC_CACHE:
        _NC_CACHE["nc"] = build_program()
    nc = _NC_CACHE["nc"]
    in_maps = _layout_inputs(inputs)
    res = run_bass_kernel_spmd(nc, in_maps, core_ids=list(range(8)))
    return _assemble(res.results)
```

```python
import math
import numpy as np
import concourse.bass as bass
import concourse.mybir as mybir
from concourse.bass_utils import run_bass_kernel_spmd
from contextlib import ExitStack

F32 = mybir.dt.float32
BF16 = mybir.dt.bfloat16
I32 = mybir.dt.int32
ALU = mybir.AluOpType
AF = mybir.ActivationFunctionType
AX = mybir.AxisListType


def _is_ap(v):
    return hasattr(v, "partition_size") and hasattr(v, "rearrange")


class Sched:
    ENG = ("pe", "dve", "act", "pool", "sp")

    def __init__(self, nc, es, n_dma_sems=48):
        self.nc = nc
        self.es = es
        self.q = {k: [] for k in self.ENG}
        self.sems = []
        self.esem = {}
        for k in ("pe", "dve", "act", "pool"):
            self.esem[k] = len(self.sems)
            self.sems.append(es.enter_context(nc.semaphore("s_" + k)))
        self.cnt = {k: 0 for k in self.esem}
        self.dma_ids = []
        self.dma_pool = {"sp": [], "pool": []}
        for i in range(n_dma_sems):
            self.dma_ids.append(len(self.sems))
            self.dma_pool["sp" if i < (2 * n_dma_sems) // 3 else "pool"].append(len(self.sems))
            self.sems.append(es.enter_context(nc.semaphore("s_dma%d" % i)))
        self.dma_cnt = {i: 0 for i in self.dma_ids}
        self.dma_rr = {"sp": 0, "pool": 0}
        self.waited = {}
        self.last_w = {}
        self.readers = {}
        self.barrier_tok = {}
        self.n_wait = 0

    def sb(self, name, shape, dtype, es=None):
        return (es or self.es).enter_context(self.nc.sbuf_tensor(name, list(shape), dtype))

    def ps(self, name, shape, dtype=F32, es=None):
        return (es or self.es).enter_context(self.nc.psum_tensor(name, list(shape), dtype))

    def barrier(self):
        tok = {}
        for k in ("pe", "dve", "act", "pool"):
            if self.cnt[k] > 0:
                tok[self.esem[k]] = self.cnt[k]
        for si in self.dma_ids:
            if self.dma_cnt[si] > 0:
                tok[si] = 16 * self.dma_cnt[si]
        self.barrier_tok = tok

    def _record(self, eng, fn, reads, writes, dma=False):
        need = dict(self.barrier_tok)

        def add(tok):
            if tok is None:
                return
            s, v = tok
            if need.get(s, 0) < v:
                need[s] = v

        for k in reads:
            add(self.last_w.get(k))
        for k in writes:
            add(self.last_w.get(k))
            for t in self.readers.get(k, ()):
                add(t)
        if dma:
            pool_ = self.dma_pool[eng]
            si = pool_[self.dma_rr[eng] % len(pool_)]
            self.dma_rr[eng] += 1
            if self.dma_cnt[si] > 0:
                add((si, 16 * self.dma_cnt[si]))
            self.dma_cnt[si] += 1
            tok = (si, 16 * self.dma_cnt[si])
            inc = (si, 16)
        else:
            self.cnt[eng] += 1
            tok = (self.esem[eng], self.cnt[eng])
            inc = (self.esem[eng], 1)
        waits = []
        for s, v in need.items():
            if eng == "pe" and s == self.esem["pe"]:
                continue
            if self.waited.get((eng, s), 0) >= v:
                continue
            self.waited[(eng, s)] = v
            waits.append((s, v))
        self.n_wait += len(waits)
        self.q[eng].append((waits, fn, inc))
        for k in writes:
            self.last_w[k] = tok
            self.readers[k] = []
        for k in reads:
            self.readers.setdefault(k, []).append(tok)
        return tok

    def op(self, eng, meth, *, r=None, w=None, xr=(), xw=(), **kw):
        reads, writes = [], []
        for k, v in kw.items():
            if _is_ap(v):
                if k in ("out", "accum_out", "ap"):
                    writes.append(v.name)
                else:
                    reads.append(v.name)
        if r is not None:
            reads = list(r)
        if w is not None:
            writes = list(w)
        reads += list(xr)
        writes += list(xw)

        def fn(e, meth=meth, kw=kw):
            return getattr(e, meth)(**kw)

        return self._record(eng, fn, reads, writes)

    def dma(self, eng, out, in_, r=None, w=None, **kw):
        reads = [in_.name] if r is None else list(r)
        writes = [out.name] if w is None else list(w)

        def fn(e, out=out, in_=in_, kw=kw):
            return e.dma_start(out=out, in_=in_, **kw)

        return self._record(eng, fn, reads, writes, dma=True)

    def emit(self):
        final_waits = []
        for si in self.dma_ids:
            if self.dma_cnt[si] > 0:
                final_waits.append((si, 16 * self.dma_cnt[si]))
        for k in ("pe", "dve", "act", "pool"):
            if self.cnt[k] > 0:
                final_waits.append((self.esem[k], self.cnt[k]))
        sems = self.sems
        q = self.q

        def mk(name):
            def body(e):
                for waits, fn, inc in q[name]:
                    for s, v in waits:
                        e.wait_ge(sems[s], v)
                    ins = fn(e)
                    ins.then_inc(sems[inc[0]], inc[1])
                if name == "sp":
                    for s, v in final_waits:
                        e.wait_ge(sems[s], v)
            return body

        with self.nc.Block() as block:
            block.tensor(mk("pe"))
            block.vector(mk("dve"))
            block.scalar(mk("act"))
            block.gpsimd(mk("pool"))
            block.sync(mk("sp"))


D = 2048
NS = 32
NT = 16
EPS = 1e-6
TWO_PI = 2.0 * math.pi
C1 = 6.28125
C2 = TWO_PI - C1
NEG = -30000.0
T5_THR = [float(b) for b in range(1, 17)] + [float(math.ceil(16.0 * 8.0 ** (k / 16.0))) for k in range(1, 16)]
NB = 4
NCHUNK = 128
NGRP = 4
DELTA = 1e-5


def build_program(upto="all", dbg_names=(), ngrp=NGRP, nblk=NCHUNK // NB):
    nc = bass.Bass("TRN2", target_bir_lowering=False)

    def din(name, shape, dt=F32):
        return nc.dram_tensor(name, list(shape), dt, kind="ExternalInput").ap()

    def dscr(name, shape, dt=BF16):
        return nc.dram_tensor(name, list(shape), dt, kind="Internal").ap()

    x_s = din("x_s", [NS, 128, D])
    pos_s = din("pos_s", [128, NS], I32)
    pos_r = din("pos_r", [NS, 128], I32)
    flags = din("flags", [128, 2])
    invf = din("invf", [128, 32])
    g1 = din("norm1_gain", [D])
    w_in = din("w_in", [D, 2112])
    qag = din("q_a_gain", [512])
    w_qb = din("w_q_b", [512, 1536])
    kvag = din("kv_a_gain", [256])
    w_kvb = din("w_kv_b", [256, 2048])
    mqg = din("mla_q_gain", [192])
    mkg = din("mla_k_gain", [192])
    sqg = din("swa_q_gain", [64])
    skg = din("swa_k_gain", [64])
    sinks = din("swa_sinks", [16])
    relt = din("rel_bias_table", [32 * 16])
    gog = din("group_out_gain", [D])
    w_out = din("w_out", [D, D])
    g2 = din("norm2_gain", [D])
    pwq = din("peer_w_q", [D, D])
    skT_d = din("skT", [128, 16, 128])
    ut_t = din("ut_t", [NCHUNK, 128, 16, 128])
    pv = din("peer_v", [128 * NCHUNK, D])
    out_h = nc.dram_tensor("out", [NT, 128, D], F32, kind="ExternalOutput").ap()

    kt_scr = dscr("kt_scr", [8, NS, 128, 128])
    kpe_scr = dscr("kpe_scr", [4, NS, 128, 128])
    v_scr = dscr("v_scr", [NS, 128, 8, 129])
    qt_scr = dscr("qt_scr", [8, NT, 128, 128])
    qpe_scr = dscr("qpe_scr", [4, NT, 128, 128])
    mix_scr = dscr("mix_scr", [NT, 128, 8, 128])
    hn_scr = dscr("hn_scr", [NGRP, 128, 16, 4, 128])
    q_scr = dscr("q_scr", [NGRP, 128, 16, 512])

    dbg_out = {}

    with ExitStack() as es:
        s = Sched(nc, es)

        def dbg(name, ap, shape, dt=F32):
            if name in dbg_names:
                t = nc.dram_tensor("dbg_" + name, list(shape), dt, kind="ExternalOutput").ap()
                s.dma("sp", t, ap)

        def bcast_load(dst, src_1d, n, eng="sp"):
            s.dma(eng, dst, src_1d.rearrange("(o n) -> o n", o=1).to_broadcast([128, n]))

        ident = s.sb("ident", [128, 128], BF16)
        tri = s.sb("tri", [128, 128], BF16)
        trif = s.sb("trif", [128, 128], F32)
        epsT = s.sb("epsT", [128, 1], F32)
        lnps = s.sb("lnps", [128, 1], F32)
        mhalf = s.sb("mhalf", [128, 16], F32)
        flg = s.sb("flg", [128, 2], F32)
        phA = ExitStack()
        cosT = s.sb("cosT", [128, NS, 32], F32, phA)
        sinT = s.sb("sinT", [128, NS, 32], F32, phA)
        posf = s.sb("posf", [128, NS], F32, phA)
        swa_kt = s.sb("swa_kt", [128, NS * 128], BF16, phA)
        swa_v = s.sb("swa_v", [128, NS, 2, 65], BF16, phA)

        with ExitStack() as ph:
            it = s.sb("it", [128, 128], F32, ph)
            s.op("pool", "iota", out=it[:], pattern=[[1, 128]], base=0, channel_multiplier=-1,
                 allow_small_or_imprecise_dtypes=True)
            s.op("dve", "tensor_single_scalar", out=trif[:], in_=it[:], scalar=0.0, op=ALU.is_equal)
            s.op("dve", "tensor_copy", out=ident[:], in_=trif[:])
            s.op("dve", "tensor_single_scalar", out=trif[:], in_=it[:], scalar=0.0, op=ALU.is_ge)
            s.op("dve", "tensor_copy", out=tri[:], in_=trif[:])
            s.op("dve", "memset", ap=epsT[:], constant=EPS)
            s.op("dve", "memset", ap=lnps[:], constant=math.log(192.0 ** -0.5))
            s.op("dve", "memset", ap=mhalf[:], constant=-0.5)
            s.op("dve", "memset", ap=swa_v[:], constant=1.0)
            s.dma("sp", flg[:], flags)
            posi = s.sb("posi", [128, NS], I32, ph)
            s.dma("sp", posi[:], pos_s)
            s.op("dve", "tensor_copy", out=posf[:], in_=posi[:])
            ivf = s.sb("ivf", [128, 32], F32, ph)
            s.dma("sp", ivf[:], invf)
            ang = s.sb("ang", [128, NS, 32], F32, ph)
            s.op("dve", "tensor_tensor", out=ang[:], in0=posf[:].unsqueeze(2).to_broadcast([128, NS, 32]),
                 in1=ivf[:].unsqueeze(1).to_broadcast([128, NS, 32]), op=ALU.mult)
            kq = s.sb("kq", [128, NS, 32], F32, ph)
            ki = s.sb("ki", [128, NS, 32], I32, ph)
            red = s.sb("red", [128, NS, 32], F32, ph)
            for which, dst in ((0, sinT), (1, cosT)):
                src = ang
                if which == 1:
                    s.op("dve", "tensor_scalar", out=red[:], in0=ang[:], scalar1=math.pi / 2, scalar2=None, op0=ALU.add)
                    src = red
                s.op("dve", "tensor_scalar", out=kq[:], in0=src[:], scalar1=1.0 / TWO_PI, scalar2=None, op0=ALU.mult)
                s.op("dve", "tensor_copy", out=ki[:], in_=kq[:])
                s.op("dve", "tensor_copy", out=kq[:], in_=ki[:])
                s.op("dve", "scalar_tensor_tensor", out=red[:], in0=kq[:], scalar=-C1, in1=src[:], op0=ALU.mult, op1=ALU.add)
                s.op("dve", "scalar_tensor_tensor", out=red[:], in0=kq[:], scalar=-C2, in1=red[:], op0=ALU.mult, op1=ALU.add)
                s.op("dve", "tensor_scalar", out=red[:], in0=red[:], scalar1=math.pi, scalar2=-math.pi, op0=ALU.min, op1=ALU.max)
                s.op("act", "activation", out=dst[:], in_=red[:], func=AF.Sin)
        s.barrier()

        def rstd_act(dst, ss, inv_n, post_scale=None):
            s.op("act", "activation", out=dst, in_=ss, func=AF.Ln, scale=inv_n, bias=epsT[:, 0:1])
            if post_scale is None:
                s.op("act", "activation", out=dst, in_=dst, func=AF.Exp, scale=-0.5)
            else:
                s.op("act", "activation", out=dst, in_=dst, func=AF.Exp, scale=-0.5, bias=lnps[:, 0:1])

        def rstd_from_ss(dst, ss, n_cols, inv_n, post_scale=None):
            s.op("pool", "tensor_scalar", out=dst, in0=ss, scalar1=inv_n, scalar2=EPS, op0=ALU.mult, op1=ALU.add)
            s.op("pool", "tensor_tensor", out=dst, in0=dst, in1=mhalf[:, 0:n_cols], op=ALU.pow)
            if post_scale is not None:
                s.op("pool", "tensor_scalar", out=dst, in0=dst, scalar1=post_scale, scalar2=None, op0=ALU.mult)

        def sumsq(dst_col, src, junk):
            s.op("dve", "scalar_tensor_tensor", out=junk, in0=src, scalar=1.0, in1=src,
                 op0=ALU.mult, op1=ALU.mult, accum_out=dst_col)

        def rope(dst1, dst2, x1, x2, cs, sn, shape, t1, t2):
            s.op("dve", "tensor_tensor", out=t1, in0=x1, in1=cs, op=ALU.mult)
            s.op("dve", "tensor_tensor", out=t2, in0=x2, in1=sn, op=ALU.mult)
            s.op("dve", "tensor_tensor", out=dst1, in0=t1, in1=t2, op=ALU.subtract)
            s.op("dve", "tensor_tensor", out=t1, in0=x2, in1=cs, op=ALU.mult)
            s.op("dve", "tensor_tensor", out=t2, in0=x1, in1=sn, op=ALU.mult)
            s.op("dve", "tensor_tensor", out=dst2, in0=t1, in1=t2, op=ALU.add)

        BT = s.sb("BT", [128, 3, 16, 128], F32, phA)
        esink = s.sb("esink", [128, 16], F32, phA)
        phT5 = ExitStack()
        tblb = s.sb("tblb", [128, 32, 16], F32, phT5)
        dtab = s.sb("dtab", [128, 32, 16], F32, phT5)
        pri = s.sb("pri", [128, 128], I32, phT5)
        prf = s.sb("prf", [128, 128], F32, phT5)
        dist = s.sb("dist", [128, 128], F32, phT5)
        ge = s.sb("ge", [128, 128], F32, phT5)
        tmpb = s.sb("tmpb", [128, 16, 128], F32, phT5)
        vm = s.sb("vm", [128, 128], F32, phT5)

        def t5_gen():
            bcast_load(tblb[:].rearrange("p a b -> p (a b)"), relt, 512)
            s.op("pool", "tensor_tensor", out=dtab[:, 1:32, :], in0=tblb[:, 1:32, :], in1=tblb[:, 0:31, :],
                 op=ALU.subtract)
            s.dma("sp", pri[:], pos_r[2:3, :].to_broadcast([128, 128]))
            s.op("pool", "tensor_copy", out=prf[:], in_=pri[:])
            yield
            for ty in (0, 1):
                if ty == 0:
                    s.op("pool", "tensor_scalar", out=dist[:], in0=prf[:], scalar1=posf[:, 2:3], scalar2=0.0,
                         op0=ALU.subtract, op1=ALU.max)
                else:
                    s.op("pool", "tensor_scalar", out=dist[:], in0=prf[:], scalar1=posf[:, 1:2], scalar2=flg[:, 1:2],
                         op0=ALU.subtract, op1=ALU.mult)
                    s.op("pool", "tensor_scalar", out=ge[:], in0=prf[:], scalar1=posf[:, 3:4], scalar2=flg[:, 0:1],
                         op0=ALU.subtract, op1=ALU.mult)
                    s.op("pool", "tensor_tensor", out=dist[:], in0=dist[:], in1=ge[:], op=ALU.add)
                    s.op("pool", "tensor_scalar", out=dist[:], in0=dist[:], scalar1=0.0, scalar2=None, op0=ALU.max)
                s.op("pool", "tensor_copy", out=BT[:, ty, :, :],
                     in_=tblb[:, 0, :].unsqueeze(2).to_broadcast([128, 16, 128]))
                yield
                for b in range(1, 32):
                    s.op("pool", "tensor_single_scalar", out=ge[:], in_=dist[:], scalar=T5_THR[b - 1], op=ALU.is_ge)
                    s.op("pool", "tensor_tensor", out=tmpb[:], in0=ge[:].unsqueeze(1).to_broadcast([128, 16, 128]),
                         in1=dtab[:, b, :].unsqueeze(2).to_broadcast([128, 16, 128]), op=ALU.mult)
                    s.op("pool", "tensor_tensor", out=BT[:, ty, :, :], in0=BT[:, ty, :, :], in1=tmpb[:], op=ALU.add)
                    yield
            s.op("pool", "tensor_scalar", out=vm[:], in0=trif[:], scalar1=-1.0, scalar2=-NEG, op0=ALU.add, op1=ALU.mult)
            s.op("pool", "tensor_tensor", out=BT[:, 0, :, :], in0=BT[:, 0, :, :],
                 in1=vm[:].unsqueeze(1).to_broadcast([128, 16, 128]), op=ALU.add)
            for ty, fcol in ((2, flg[:, 0:1]), (1, flg[:, 1:2])):
                s.op("pool", "tensor_scalar", out=vm[:], in0=trif[:], scalar1=-1.0, scalar2=-1.0,
                     op0=ALU.mult, op1=ALU.subtract)
                s.op("pool", "tensor_scalar", out=vm[:], in0=vm[:], scalar1=fcol, scalar2=-1.0, op0=ALU.mult, op1=ALU.add)
                s.op("pool", "tensor_scalar", out=vm[:], in0=vm[:], scalar1=-NEG, scalar2=None, op0=ALU.mult)
                s.op("pool", "tensor_tensor", out=BT[:, ty, :, :], in0=BT[:, 1, :, :],
                     in1=vm[:].unsqueeze(1).to_broadcast([128, 16, 128]), op=ALU.add)
            yield

        t5 = t5_gen()

        def t5_step(n):
            for _ in range(n):
                try:
                    next(t5)
                except StopIteration:
                    return

        def interleave(gens, depth):
            it = iter(gens)
            active = []
            done = False
            while True:
                while not done and len(active) < depth:
                    g_ = next(it, None)
                    if g_ is None:
                        done = True
                        break
                    active.append(g_)
                if not active:
                    break
                for g_ in list(active):
                    try:
                        next(g_)
                    except StopIteration:
                        active.remove(g_)

        with ExitStack() as ph:
            w_kv = s.sb("w_kv", [128, 16, 576], BF16, ph)
            wv = w_in.rearrange("(k p) c -> p k c", p=128)
            for k0 in range(0, 16, 4):
                s.dma("pool", w_kv[:, k0:k0 + 4, 0:320], wv[:, k0:k0 + 4, 512:832])
                s.dma("pool", w_kv[:, k0:k0 + 4, 320:576], wv[:, k0:k0 + 4, 1856:2112])
            w_kvb_sb = s.sb("w_kvb_sb", [128, 2, 2048], BF16, ph)
            s.dma("pool", w_kvb_sb[:], w_kvb.rearrange("(k p) c -> p k c", p=128))
            g1b = s.sb("g1b", [128, D], F32, ph)
            bcast_load(g1b[:], g1, D)
            kvagb = s.sb("kvagb", [128, 256], F32, ph)
            bcast_load(kvagb[:], kvag, 256)
            gkpe = s.sb("gkpe", [128, 64], F32, ph)
            bcast_load(gkpe[:], mkg[128:192], 64)
            junk = s.sb("junk", [128, D], BF16, ph)

            def two(name, shape, dt):
                return [s.sb("%s_%d" % (name, i), shape, dt, ph) for i in range(2)]

            xt_ = two("xt", [128, D], F32)
            n1b_ = two("n1b", [128, D], BF16)
            n1T_ = two("n1T", [128, 16, 128], BF16)
            ss1_ = two("ss1", [128, 4], F32)
            r1_ = two("r1", [128, 4], F32)
            pab_ = two("pab", [128, 576], F32)
            kvn_ = two("kvn", [128, 256], BF16)
            kvnT_ = two("kvnT", [128, 2, 128], BF16)
            kvf_ = two("kvf", [128, 8, 256], F32)
            sq_ = two("sq", [128, 8, 128], F32)
            ssk_ = two("ssk", [128, 8], F32)
            rk_ = two("rk", [128, 8], F32)
            kpg_ = two("kpg", [128, 64], F32)
            kpr_ = two("kpr", [128, 64], F32)
            tA_ = two("tA", [128, 32], F32)
            tB_ = two("tB", [128, 32], F32)
            kn_b_ = two("kn_b", [128, 8, 128], BF16)
            kp_b_ = two("kp_b", [128, 8, 64], BF16)
            ktile = two("ktile", [128, 8, 128], BF16)
            kptile = two("kptile", [128, 4, 128], BF16)
            vtile = two("vtile", [128, 8, 129], BF16)
            ksb_ = two("ksb", [128, 128], BF16)
            ptrA = s.ps("ptrA", [128, 2048], BF16, ph)
            ptrB = s.ps("ptrB", [128, 2048], BF16, ph)
            pA1 = s.ps("pA1", [128, 512], F32, ph)
            pB1 = s.ps("pB1", [128, 512], F32, ph)
            pkv = s.ps("pkv", [128, 1024], F32, ph)
            for i in range(2):
                s.op("dve", "memset", ap=vtile[i][:], constant=1.0)

            t5_step(1000)

            def a1_tile(sl):
                par = sl % 2
                xb, n1b, n1T, ss1, r1, pab = xt_[par], n1b_[par], n1T_[par], ss1_[par], r1_[par], pab_[par]
                kvn, kvnT, kvf, sq, ssk, rk = kvn_[par], kvnT_[par], kvf_[par], sq_[par], ssk_[par], rk_[par]
                kpg, kpr, tA, tB, kn_b, kp_b, ksb = kpg_[par], kpr_[par], tA_[par], tB_[par], kn_b_[par], kp_b_[par], ksb_[par]
                s.dma("sp", xb[:], x_s[sl])
                sumsq(ss1[:, 0:1], xb[:], junk[:])
                rstd_act(r1[:, 0:1], ss1[:, 0:1], 1.0 / D)
                s.op("dve", "scalar_tensor_tensor", out=n1b[:], in0=xb[:], scalar=r1[:, 0:1], in1=g1b[:],
                     op0=ALU.mult, op1=ALU.mult)
                yield
                for kc in range(16):
                    s.op("pe", "transpose", out=ptrA[:, kc * 128:(kc + 1) * 128], in_=n1b[:, kc * 128:(kc + 1) * 128],
                         identity=ident[:])
                s.op("act", "copy", out=n1T[:].rearrange("p a b -> p (a b)"), in_=ptrA[:])
                yield
                for kc in range(16):
                    s.op("pe", "matmul", out=pA1[:, 0:320], lhsT=n1T[:, kc, :], rhs=w_kv[:, kc, 0:320],
                         start=(kc == 0), stop=(kc == 15))
                for kc in range(16):
                    s.op("pe", "matmul", out=pB1[:, 0:256], lhsT=n1T[:, kc, :], rhs=w_kv[:, kc, 320:576],
                         start=(kc == 0), stop=(kc == 15))
                s.op("act", "copy", out=pab[:, 0:320], in_=pA1[:, 0:320])
                s.op("act", "copy", out=pab[:, 320:576], in_=pB1[:, 0:256])
                yield
                sumsq(ss1[:, 1:2], pab[:, 0:256], junk[:, 0:256])
                rstd_act(r1[:, 1:2], ss1[:, 1:2], 1.0 / 256)
                s.op("dve", "scalar_tensor_tensor", out=kvn[:], in0=pab[:, 0:256], scalar=r1[:, 1:2], in1=kvagb[:],
                     op0=ALU.mult, op1=ALU.mult)
                ks3 = pab[:, 320:448].rearrange("p (a b) -> p a b", a=2)
                s.op("dve", "tensor_tensor", out=sq[:, 0:2, 0:64], in0=ks3, in1=ks3, op=ALU.mult)
                s.op("dve", "tensor_reduce", out=ss1[:, 2:4], in_=sq[:, 0:2, 0:64], axis=AX.X, op=ALU.add)
                rstd_act(r1[:, 2:4], ss1[:, 2:4], 1.0 / 64)
                s.op("dve", "tensor_tensor", out=ksb[:].rearrange("p (a b) -> p a b", a=2), in0=ks3,
                     in1=r1[:, 2:4].unsqueeze(2).to_broadcast([128, 2, 64]), op=ALU.mult)
                s.op("act", "copy", out=swa_v[:, sl, :, 0:64],
                     in_=pab[:, 448:576].rearrange("p (a b) -> p a b", a=2))
                yield
                for kc in range(2):
                    s.op("pe", "transpose", out=ptrB[:, kc * 128:(kc + 1) * 128], in_=kvn[:, kc * 128:(kc + 1) * 128],
                         identity=ident[:])
                s.op("pe", "transpose", out=ptrB[:, 256:384], in_=ksb[:], identity=ident[:])
                s.op("act", "copy", out=kvnT[:].rearrange("p a b -> p (a b)"), in_=ptrB[:, 0:256])
                s.op("act", "copy", out=swa_kt[:, sl * 128:(sl + 1) * 128], in_=ptrB[:, 256:384])
                yield
                kvf_flat = kvf[:].rearrange("p a b -> p (a b)")
                for hf in range(2):
                    for cg in range(2):
                        for kc in range(2):
                            c0_ = hf * 1024 + cg * 512
                            s.op("pe", "matmul", out=pkv[:, cg * 512:(cg + 1) * 512], lhsT=kvnT[:, kc, :],
                                 rhs=w_kvb_sb[:, kc, c0_:c0_ + 512], start=(kc == 0), stop=(kc == 1))
                    if hf == 0:
                        s.op("act", "copy", out=kvf_flat[:, 0:1024], in_=pkv[:])
                    else:
                        s.op("dve", "tensor_copy", out=kvf_flat[:, 1024:2048], in_=pkv[:])
                    yield
                s.op("dve", "tensor_tensor", out=sq[:], in0=kvf[:, :, 0:128], in1=kvf[:, :, 0:128], op=ALU.mult)
                s.op("dve", "tensor_reduce", out=ssk[:], in_=sq[:], axis=AX.X, op=ALU.add)
                sumsq(ss1[:, 2:3], pab[:, 256:320], junk[:, 0:64])
                s.op("dve", "tensor_scalar", out=ssk[:], in0=ssk[:], scalar1=ss1[:, 2:3], scalar2=None, op0=ALU.add)
                rstd_act(rk[:], ssk[:], 1.0 / 192, post_scale=192.0 ** -0.5)
                vt_ = vtile[par]
                s.op("act", "copy", out=vt_[:, :, 0:128], in_=kvf[:, :, 128:256])
                s.dma("sp", v_scr[sl], vt_[:], w=[("v", sl)])
                s.op("dve", "tensor_tensor", out=kpg[:], in0=pab[:, 256:320], in1=gkpe[:], op=ALU.mult)
                rope(kpr[:, 0:32], kpr[:, 32:64], kpg[:, 0:32], kpg[:, 32:64], cosT[:, sl, :], sinT[:, sl, :],
                     None, tA[:], tB[:])
                yield
                s.op("dve", "tensor_tensor", out=kn_b[:], in0=kvf[:, :, 0:128],
                     in1=rk[:].unsqueeze(2).to_broadcast([128, 8, 128]), op=ALU.mult)
                s.op("dve", "tensor_tensor", out=kp_b[:], in0=kpr[:].unsqueeze(1).to_broadcast([128, 8, 64]),
                     in1=rk[:].unsqueeze(2).to_broadcast([128, 8, 64]), op=ALU.mult)
                yield
                for h in range(8):
                    s.op("pe", "transpose", out=ptrB[:, 512 + h * 128:512 + (h + 1) * 128], in_=kn_b[:, h, :], identity=ident[:])
                for pr in range(4):
                    s.op("pe", "transpose", out=ptrB[:, 1536 + pr * 128:1536 + (pr + 1) * 128],
                         in_=kp_b[:, 2 * pr:2 * pr + 2, :].rearrange("p a b -> p (a b)"), identity=ident[:])
                kt_ = ktile[par]
                kpt_ = kptile[par]
                s.op("act", "copy", out=kt_[:].rearrange("p a b -> p (a b)"), in_=ptrB[:, 512:1536])
                s.op("act", "copy", out=kpt_[:].rearrange("p a b -> p (a b)"), in_=ptrB[:, 1536:2048])
                s.dma("sp", kt_scr[:, sl].rearrange("h d t -> d h t"), kt_[:], w=[("kt", sl)])
                s.dma("sp", kpe_scr[:, sl].rearrange("h d t -> d h t"), kpt_[:], w=[("kpe", sl)])
                if sl == 0:
                    dbg("kvf", kvf[:], [128, 8, 256])
                    dbg("rk", rk[:], [128, 8])
                    dbg("kpr", kpr[:], [128, 64])

            interleave((a1_tile(sl) for sl in range(NS)), 2)
        t5_step(1000)
        dbg("BT", BT[:], [128, 3, 16, 128])
        s.barrier()
        phT5.close()
        if upto == "A1":
            s.emit()
            phA.close()
            return nc

        with ExitStack() as ph:
            w_q = s.sb("w_q", [128, 16, 1536], BF16, ph)
            wv = w_in.rearrange("(k p) c -> p k c", p=128)
            for k0 in range(0, 16, 4):
                s.dma("pool", w_q[:, k0:k0 + 4, 0:512], wv[:, k0:k0 + 4, 0:512])
                s.dma("pool", w_q[:, k0:k0 + 4, 512:1536], wv[:, k0:k0 + 4, 832:1856])
            w_qb_sb = s.sb("w_qb_sb", [128, 4, 1536], BF16, ph)
            s.dma("pool", w_qb_sb[:], w_qb.rearrange("(k p) c -> p k c", p=128))
            g1b = s.sb("g1b2", [128, D], F32, ph)
            bcast_load(g1b[:], g1, D)
            qagb = s.sb("qagb", [128, 512], F32, ph)
            bcast_load(qagb[:], qag, 512)
            Gq = s.sb("Gq", [128, 192], F32, ph)
            bcast_load(Gq[:], mqg, 192)
            gtmp = s.sb("gtmp", [128, 192], F32, ph)
            bcast_load(gtmp[:], mkg, 192)
            s.op("dve", "tensor_tensor", out=Gq[:, 0:128], in0=Gq[:, 0:128], in1=gtmp[:, 0:128], op=ALU.mult)
            Gs = s.sb("Gs", [128, 64], F32, ph)
            bcast_load(Gs[:], sqg, 64)
            bcast_load(gtmp[:, 0:64], skg, 64)
            s.op("dve", "scalar_tensor_tensor", out=Gs[:], in0=Gs[:], scalar=0.125, in1=gtmp[:, 0:64],
                 op0=ALU.mult, op1=ALU.mult)
            gob = s.sb("gob_s", [128, 1024], F32, ph)
            bcast_load(gob[:], gog[1024:2048], 1024)
            bcast_load(esink[:], sinks, 16)
            s.op("act", "activation", out=esink[:], in_=esink[:], func=AF.Exp)
            s.barrier()
            xt = [s.sb("xu0", [128, D], F32, ph)] * 2
            junk = s.sb("junk2", [128, D], BF16, ph)
            n1b = s.sb("n1b2", [128, D], BF16, ph)
            n1T = s.sb("n1T2", [128, 16, 128], BF16, ph)
            ss1 = s.sb("ss2", [128, 4], F32, ph)
            r1 = s.sb("r2", [128, 4], F32, ph)
            pq = s.sb("pq", [128, 1536], F32, ph)
            qln = s.sb("qln", [128, 512], BF16, ph)
            qlT = s.sb("qlT", [128, 4, 128], BF16, ph)
            qf = s.sb("qf", [128, 8, 192], F32, ph)
            sq_raw = s.sb("sq2", [128, 1536], F32, ph)
            sq = sq_raw[:].rearrange("p (a b) -> p a b", a=8)
            qsn = sq_raw[:, 0:1024].rearrange("p (a b) -> p a b", a=16)
            ssq = s.sb("ssq", [128, 16], F32, ph)
            rq = s.sb("rq", [128, 16], F32, ph)
            qn_b = s.sb("qn_b", [128, 8, 128], BF16, ph)
            qp_b = s.sb("qp_b", [128, 8, 64], BF16, ph)
            tA = s.sb("tA2", [128, 8, 32], F32, ph)
            tB = s.sb("tB2", [128, 8, 32], F32, ph)
            qtile = [s.sb("qtile%d" % i, [128, 8, 128], BF16, ph) for i in range(2)]
            qptile = [s.sb("qptile%d" % i, [128, 4, 128], BF16, ph) for i in range(2)]
            qsb = s.sb("qsb", [128, 8, 2, 64], BF16, ph)
            qsT = s.sb("qsT", [128, 8, 128], BF16, ph)
            sbias = s.sb("sbias", [128, 8, 128], F32, ph)
            pT3 = [s.sb("pT3_%d" % i, [128, 8, 128], BF16, ph) for i in range(3)]
            den = s.sb("den", [128, 8], F32, ph)
            osw = s.sb("osw", [128, 16, 64], F32, ph)
            osn = s.sb("osn", [128, 1024], BF16, ph)
            mtile = [s.sb("mtile%d" % i, [128, 8, 128], BF16, ph) for i in range(2)]
            ptr = s.ps("ptr2", [128, 2048], BF16, ph)
            pA = s.ps("pA", [128, 1536], F32, ph)
            pB = s.ps("pB", [128, 1024], F32, ph)

            ptrS = s.ps("ptrS", [128, 1024], BF16, ph)
            qsn_t = s.sb("qsn_t", [128, 16, 64], F32, ph)
            ss_m = s.sb("ss_m", [128, 2], F32, ph)
            r_m = s.sb("r_m", [128, 2], F32, ph)
            ssq_s = s.sb("ssq_s", [128, 16], F32, ph)
            rq_s = s.sb("rq_s", [128, 16], F32, ph)
            ss_s = s.sb("ss_s", [128, 2], F32, ph)
            r_s = s.sb("r_s", [128, 2], F32, ph)
            junk_s = s.sb("junk_s", [128, 1024], BF16, ph)

            def a2_front(j):
                sl = 2 * j
                xb = xt[j % 2]
                s.dma("sp", xb[:], x_s[sl])
                sumsq(ss1[:, 0:1], xb[:], junk[:])
                rstd_from_ss(r1[:, 0:1], ss1[:, 0:1], 1, 1.0 / D)
                s.op("dve", "scalar_tensor_tensor", out=n1b[:], in0=xb[:], scalar=r1[:, 0:1], in1=g1b[:],
                     op0=ALU.mult, op1=ALU.mult)
                yield
                for kc in range(16):
                    s.op("pe", "transpose", out=ptr[:, kc * 128:(kc + 1) * 128], in_=n1b[:, kc * 128:(kc + 1) * 128],
                         identity=ident[:])
                s.op("act", "copy", out=n1T[:].rearrange("p a b -> p (a b)"), in_=ptr[:])
                yield
                yield
                for kc in range(16):
                    s.op("pe", "matmul", out=pA[:, 0:512], lhsT=n1T[:, kc, :], rhs=w_q[:, kc, 0:512],
                         start=(kc == 0), stop=(kc == 15))
                s.op("act", "copy", out=pq[:, 0:512], in_=pA[:, 0:512])
                yield
                for cg in range(2):
                    for kc in range(16):
                        s.op("pe", "matmul", out=pB[:, cg * 512:(cg + 1) * 512], lhsT=n1T[:, kc, :],
                             rhs=w_q[:, kc, 512 + cg * 512:512 + (cg + 1) * 512], start=(kc == 0), stop=(kc == 15))
                s.op("act", "copy", out=pq[:, 512:1536], in_=pB[:])

            def a2_mla(j):
                sl = 2 * j
                sumsq(ss_m[:, 0:1], pq[:, 0:512], junk[:, 0:512])
                rstd_from_ss(r_m[:, 0:1], ss_m[:, 0:1], 1, 1.0 / 512)
                s.op("dve", "scalar_tensor_tensor", out=qln[:], in0=pq[:, 0:512], scalar=r_m[:, 0:1], in1=qagb[:],
                     op0=ALU.mult, op1=ALU.mult)
                yield
                for kc in range(4):
                    s.op("pe", "transpose", out=ptr[:, kc * 128:(kc + 1) * 128], in_=qln[:, kc * 128:(kc + 1) * 128],
                         identity=ident[:])
                s.op("act", "copy", out=qlT[:].rearrange("p a b -> p (a b)"), in_=ptr[:, 0:512])
                yield
                for cg in range(3):
                    for kc in range(4):
                        s.op("pe", "matmul", out=pA[:, cg * 512:(cg + 1) * 512], lhsT=qlT[:, kc, :],
                             rhs=w_qb_sb[:, kc, cg * 512:(cg + 1) * 512], start=(kc == 0), stop=(kc == 3))
                s.op("act", "copy", out=qf[:].rearrange("p a b -> p (a b)"), in_=pA[:])
                yield
                s.op("dve", "tensor_tensor", out=sq[:], in0=qf[:], in1=qf[:], op=ALU.mult)
                s.op("dve", "tensor_reduce", out=ssq[:, 0:8], in_=sq[:], axis=AX.X, op=ALU.add)
                rstd_from_ss(rq[:, 0:8], ssq[:, 0:8], 8, 1.0 / 192)
                yield
                s.op("dve", "tensor_tensor", out=qf[:], in0=qf[:], in1=rq[:, 0:8].unsqueeze(2).to_broadcast([128, 8, 192]),
                     op=ALU.mult)
                s.op("dve", "tensor_tensor", out=qf[:], in0=qf[:], in1=Gq[:].unsqueeze(1).to_broadcast([128, 8, 192]),
                     op=ALU.mult)
                s.op("pool", "tensor_copy", out=qn_b[:], in_=qf[:, :, 0:128])
                csb = cosT[:, sl, :].unsqueeze(1).to_broadcast([128, 8, 32])
                snb = sinT[:, sl, :].unsqueeze(1).to_broadcast([128, 8, 32])
                rope(qp_b[:, :, 0:32], qp_b[:, :, 32:64], qf[:, :, 128:160], qf[:, :, 160:192], csb, snb, None,
                     tA[:], tB[:])
                yield
                for h in range(8):
                    s.op("pe", "transpose", out=ptr[:, h * 128:(h + 1) * 128], in_=qn_b[:, h, :], identity=ident[:])
                for pr in range(4):
                    s.op("pe", "transpose", out=ptr[:, 1024 + pr * 128:1024 + (pr + 1) * 128],
                         in_=qp_b[:, 2 * pr:2 * pr + 2, :].rearrange("p a b -> p (a b)"), identity=ident[:])
                qt_ = qtile[j % 2]
                qpt_ = qptile[j % 2]
                s.op("act", "copy", out=qt_[:].rearrange("p a b -> p (a b)"), in_=ptr[:, 0:1024])
                s.op("act", "copy", out=qpt_[:].rearrange("p a b -> p (a b)"), in_=ptr[:, 1024:1536])
                s.dma("sp", qt_scr[:, j].rearrange("h d t -> d h t"), qt_[:], w=[("qt", j)])
                s.dma("sp", qpe_scr[:, j].rearrange("h d t -> d h t"), qpt_[:], w=[("qpe", j)])

            def a2_swa(j):
                qs3 = pq[:, 512:1536].rearrange("p (a b) -> p a b", a=16)
                s.op("dve", "tensor_tensor", out=qsn_t[:], in0=qs3, in1=qs3, op=ALU.mult)
                s.op("dve", "tensor_reduce", out=ssq_s[:], in_=qsn_t[:], axis=AX.X, op=ALU.add)
                rstd_from_ss(rq_s[:], ssq_s[:], 16, 1.0 / 64)
                yield
                s.op("dve", "tensor_tensor", out=qsn_t[:], in0=qs3, in1=rq_s[:].unsqueeze(2).to_broadcast([128, 16, 64]),
                     op=ALU.mult)
                s.op("dve", "tensor_tensor", out=qsb[:].rearrange("p i g d -> p g i d"),
                     in0=qsn_t[:].rearrange("p (g i) d -> p g i d", g=2),
                     in1=Gs[:].unsqueeze(1).unsqueeze(1).to_broadcast([128, 2, 8, 64]), op=ALU.mult)
                yield
                for i in range(8):
                    s.op("pe", "transpose", out=ptrS[:, i * 128:(i + 1) * 128],
                         in_=qsb[:, i, :, :].rearrange("p a b -> p (a b)"), identity=ident[:])
                s.op("act", "copy", out=qsT[:].rearrange("p a b -> p (a b)"), in_=ptrS[:])
                yield
                types = [(0, 2 * j), (2, 2 * j + 1)] + ([(1, 2 * j - 1)] if j >= 1 else [])
                pO = pA[:, 0:1024].rearrange("p (a b) -> p a b", a=8)
                for g in range(2):
                    for ti, (ty, kslot) in enumerate(types):
                        for half in range(2):
                            s.op("pe", "matmul", out=pB[:, half * 512:(half + 1) * 512],
                                 lhsT=swa_kt[64 * g:64 * g + 64, kslot * 128:(kslot + 1) * 128],
                                 rhs=qsT[64 * g:64 * g + 64, half * 4:half * 4 + 4, :].rearrange("p a b -> p (a b)"),
                                 start=True, stop=True)
                        s.op("dve", "tensor_tensor", out=sbias[:].rearrange("p a b -> p (a b)"), in0=pB[:],
                             in1=BT[:, ty, g * 8:(g + 1) * 8, :].rearrange("p a b -> p (a b)"), op=ALU.add)
                        s.op("act", "activation", out=pT3[ti][:], in_=sbias[:], func=AF.Exp)
                        yield
                    for i in range(8):
                        for ti, (ty, kslot) in enumerate(types):
                            s.op("pe", "matmul", out=pO[:, i, 0:65], lhsT=pT3[ti][:, i, :], rhs=swa_v[:, kslot, g, :],
                                 start=(ti == 0), stop=(ti == len(types) - 1))
                    s.op("dve", "tensor_tensor", out=den[:], in0=pO[:, :, 64], in1=esink[:, g * 8:(g + 1) * 8], op=ALU.add)
                    s.op("dve", "reciprocal", out=den[:], in_=den[:])
                    s.op("dve", "tensor_tensor", out=osw[:, g * 8:(g + 1) * 8, :], in0=pO[:, :, 0:64],
                         in1=den[:].unsqueeze(2).to_broadcast([128, 8, 64]), op=ALU.mult)
                    yield
                osw_f = osw[:].rearrange("p a b -> p (a b)")
                if j == 1:
                    dbg("osw", osw_f, [128, 1024])
                sumsq(ss_s[:, 0:1], osw_f, junk_s[:])
                rstd_from_ss(r_s[:, 0:1], ss_s[:, 0:1], 1, 1.0 / 1024)
                s.op("dve", "scalar_tensor_tensor", out=osn[:], in0=osw_f, scalar=r_s[:, 0:1], in1=gob[:],
                     op0=ALU.mult, op1=ALU.mult)
                yield
                for i in range(8):
                    s.op("pe", "transpose", out=ptrS[:, i * 128:(i + 1) * 128], in_=osn[:, i * 128:(i + 1) * 128],
                         identity=ident[:])
                mt_ = mtile[j % 2]
                s.op("act", "copy", out=mt_[:].rearrange("p a b -> p (a b)"), in_=ptrS[:])
                s.dma("sp", mix_scr[j], mt_[:], w=[("mix", j)])

            for _ in a2_front(0):
                pass
            for j in range(NT):
                gens = [a2_mla(j), a2_swa(j)]
                if j + 1 < NT:
                    gens.append(a2_front(j + 1))
                interleave(iter(gens), 3)
        s.barrier()
        phA.close()
        if upto == "A2":
            s.emit()
            return nc

        with ExitStack() as phB:
            o_all = s.sb("o_all", [128, NT, 8, 128], F32, phB)
            with ExitStack() as ph:
                KT = [s.sb("KT%d" % i, [128, NS, 128], BF16, ph) for i in range(2)]
                VV = [s.sb("VV%d" % i, [128, NS, 129], BF16, ph) for i in range(2)]
                QT = [s.sb("QT%d" % i, [128, NT, 128], BF16, ph) for i in range(2)]
                KP = [s.sb("KP%d" % i, [128, NS, 128], BF16, ph) for i in range(2)]
                QP = [s.sb("QP%d" % i, [128, NT, 128], BF16, ph) for i in range(2)]
                pT = [s.sb("pT%d" % i, [128, 4, 128], BF16, ph) for i in range(6)]
                rec = s.sb("rec", [128, 1], F32, ph)
                pS = [s.ps("pS%d" % i, [128, 512], F32, ph) for i in range(5)]
                pO = [s.ps("pO%d" % i, [128, 512], F32, ph) for i in range(3)]
                cstate = [0]
                for h in range(8):
                    b_ = h % 2
                    hb = (h % 2) * 64
                    s.dma("sp", KT[b_][:], kt_scr[h].rearrange("s d t -> d s t"), r=[("kt", i) for i in range(NS)])
                    s.dma("sp", VV[b_][:], v_scr[:, :, h, :].rearrange("s k c -> k s c"), r=[("v", i) for i in range(NS)])
                    s.dma("sp", QT[b_][:], qt_scr[h].rearrange("s d t -> d s t"), r=[("qt", i) for i in range(NT)])
                    s.dma("sp", KP[b_][:], kpe_scr[h // 2].rearrange("s d t -> d s t"), r=[("kpe", i) for i in range(NS)])
                    s.dma("sp", QP[b_][:], qpe_scr[h // 2].rearrange("s d t -> d s t"), r=[("qpe", i) for i in range(NT)])
                    chunks = []
                    for j in range(NT):
                        nsl = 2 * j + 2
                        for c0 in range(0, nsl, 4):
                            chunks.append((j, c0, min(4, nsl - c0), nsl))

                    def emit_scores(ch):
                        j, c0, cn, nsl = ch
                        nonlocal_c = cstate
                        ps_ = pS[nonlocal_c[0] % 5]
                        pt_ = pT[nonlocal_c[0] % 6]
                        nonlocal_c[0] += 1
                        for si in range(cn):
                            sl = c0 + si
                            s.op("pe", "matmul", out=ps_[:, si * 128:(si + 1) * 128], lhsT=KT[b_][:, sl, :],
                                 rhs=QT[b_][:, j, :], start=True, stop=False)
                            s.op("pe", "matmul", out=ps_[:, si * 128:(si + 1) * 128], lhsT=KP[b_][hb:hb + 64, sl, :],
                                 rhs=QP[b_][hb:hb + 64, j, :], start=False, stop=True)
                        s.op("act", "activation", out=pt_[:, 0:cn, :].rearrange("p a b -> p (a b)"),
                             in_=ps_[:, 0:cn * 128], func=AF.Exp)
                        for si in range(cn):
                            sl = c0 + si
                            if sl == 2 * j:
                                s.op("pool", "tensor_tensor", out=pt_[:, si, :], in0=pt_[:, si, :], in1=tri[:], op=ALU.mult)
                            elif sl == 2 * j + 1:
                                s.op("pool", "tensor_scalar", out=pt_[:, si, :], in0=pt_[:, si, :], scalar1=flg[:, 0:1],
                                     scalar2=None, op0=ALU.mult)
                        return pt_

                    def emit_pv(ch, pt_):
                        j, c0, cn, nsl = ch
                        po = pO[j % 3]
                        for si in range(cn):
                            sl = c0 + si
                            s.op("pe", "matmul", out=po[:, 0:129], lhsT=pt_[:, si, :], rhs=VV[b_][:, sl, :],
                                 start=(sl == 0), stop=(sl == nsl - 1))
                        if c0 + cn == nsl:
                            s.op("dve", "reciprocal", out=rec[:], in_=po[:, 128:129])
                            s.op("dve", "tensor_scalar", out=o_all[:, j, h, :], in0=po[:, 0:128], scalar1=rec[:, 0:1],
                                 scalar2=None, op0=ALU.mult)

                    LOOK = 3
                    pend = []
                    for ci_, ch in enumerate(chunks):
                        pend.append((ch, emit_scores(ch)))
                        if len(pend) > LOOK:
                            c_, p_ = pend.pop(0)
                            emit_pv(c_, p_)
                    while pend:
                        c_, p_ = pend.pop(0)
                        emit_pv(c_, p_)
            s.barrier()
            dbg("o_all1", o_all[:, 1, :, :], [128, 8, 128])
            with ExitStack() as ph:
                w_o = s.sb("w_o", [128, 16, D], BF16, ph)
                wv = w_out.rearrange("(k p) c -> p k c", p=128)
                for k0 in range(0, 16, 2):
                    s.dma("pool", w_o[:, k0:k0 + 2, :], wv[:, k0:k0 + 2, :])
                gob = s.sb("gob_m", [128, 1024], F32, ph)
                bcast_load(gob[:], gog[0:1024], 1024)
                g2b = s.sb("g2b", [128, D], F32, ph)
                bcast_load(g2b[:], g2, D)
                xt = [s.sb("xv%d" % i, [128, D], F32, ph) for i in range(2)]
                junk = s.sb("junk3", [128, D], BF16, ph)
                ss3_ = [s.sb("ss3_%d" % i, [128, 2], F32, ph) for i in range(2)]
                r3_ = [s.sb("r3_%d" % i, [128, 2], F32, ph) for i in range(2)]
                omn_ = [s.sb("omn%d" % i, [128, 1024], BF16, ph) for i in range(2)]
                mixT = [s.sb("mixT%d" % i, [128, 16, 128], BF16, ph) for i in range(2)]
                hf = [s.sb("hf%d" % i, [128, D], F32, ph) for i in range(2)]
                hnb_ = [s.sb("hnb%d" % i, [128, D], BF16, ph) for i in range(2)]
                hnT = [s.sb("hnT%d" % i, [128, 16, 128], BF16, ph) for i in range(2)]
                ptr = s.ps("ptr3", [128, 1024], BF16, ph)
                ptr4 = s.ps("ptr4", [128, 2048], BF16, ph)
                pH = s.ps("pH", [128, 2048], F32, ph)

                def b2_tile(j):
                    par = j % 2
                    ss1, r1, omn, hnb = ss3_[par], r3_[par], omn_[par], hnb_[par]
                    xb = xt[par]
                    s.dma("sp", xb[:], x_s[2 * j])
                    mx = mixT[par]
                    s.dma("sp", mx[:, 8:16, :], mix_scr[j], r=[("mix", j)])
                    om = o_all[:, j, :, :].rearrange("p a b -> p (a b)")
                    sumsq(ss1[:, 0:1], om, junk[:, 0:1024])
                    rstd_from_ss(r1[:, 0:1], ss1[:, 0:1], 1, 1.0 / 1024)
                    s.op("dve", "scalar_tensor_tensor", out=omn[:], in0=om, scalar=r1[:, 0:1], in1=gob[:],
                         op0=ALU.mult, op1=ALU.mult)
                    yield
                    for i in range(8):
                        s.op("pe", "transpose", out=ptr[:, i * 128:(i + 1) * 128], in_=omn[:, i * 128:(i + 1) * 128],
                             identity=ident[:])
                    s.op("act", "copy", out=mx[:, 0:8, :].rearrange("p a b -> p (a b)"), in_=ptr[:])
                    yield
                    for cg in range(4):
                        for kc in range(16):
                            s.op("pe", "matmul", out=pH[:, cg * 512:(cg + 1) * 512], lhsT=mx[:, kc, :],
                                 rhs=w_o[:, kc, cg * 512:(cg + 1) * 512], start=(kc == 0), stop=(kc == 15))
                    hb_ = hf[par]
                    s.op("dve", "tensor_tensor", out=hb_[:], in0=pH[:], in1=xb[:], op=ALU.add)
                    s.dma("sp", out_h[j], hb_[:], w=[("h", j)])
                    yield
                    sumsq(ss1[:, 1:2], hb_[:], junk[:])
                    rstd_from_ss(r1[:, 1:2], ss1[:, 1:2], 1, 1.0 / D)
                    s.op("dve", "scalar_tensor_tensor", out=hnb[:], in0=hb_[:], scalar=r1[:, 1:2], in1=g2b[:],
                         op0=ALU.mult, op1=ALU.mult)
                    yield
                    for kc in range(16):
                        s.op("pe", "transpose", out=ptr4[:, kc * 128:(kc + 1) * 128], in_=hnb[:, kc * 128:(kc + 1) * 128],
                             identity=ident[:])
                    ht_ = hnT[par]
                    s.op("act", "copy", out=ht_[:].rearrange("p a b -> p (a b)"), in_=ptr4[:])
                    s.dma("sp", hn_scr[j // 4][:, :, j % 4, :], ht_[:], w=[("hn", j)])

                interleave((b2_tile(j) for j in range(NT)), 2)
        s.barrier()
        if upto == "B":
            s.emit()
            return nc

        with ExitStack() as ph:
            w_pq = s.sb("w_pq", [128, 16, D], BF16, ph)
            wv = pwq.rearrange("(k p) c -> p k c", p=128)
            for k0 in range(0, 16, 2):
                s.dma("pool", w_pq[:, k0:k0 + 2, :], wv[:, k0:k0 + 2, :])
            hg = [s.sb("hg%d" % i, [128, 16, 4, 128], BF16, ph) for i in range(2)]
            qg = [s.sb("qg%d" % i, [128, 16, 512], BF16, ph) for i in range(2)]
            pQ = [s.ps("pQ%d" % i, [128, 512], F32, ph) for i in range(2)]
            for g in range(NGRP):
                hg_ = hg[g % 2]
                qg_ = qg[g % 2]
                s.dma("sp", hg_[:], hn_scr[g], r=[("hn", 4 * g + i) for i in range(4)])
                for c in range(16):
                    pq_ = pQ[c % 2]
                    for kc in range(16):
                        s.op("pe", "matmul", out=pq_[:], lhsT=w_pq[:, kc, c * 128:(c + 1) * 128],
                             rhs=hg_[:, kc, :, :].rearrange("p a b -> p (a b)"), start=(kc == 0), stop=(kc == 15))
                    if c % 2 == 0:
                        s.op("act", "copy", out=qg_[:, c, :], in_=pq_[:])
                    else:
                        s.op("dve", "tensor_copy", out=qg_[:, c, :], in_=pq_[:])
                s.dma("sp", q_scr[g], qg_[:], w=[("q", g)])
        s.barrier()

        if upto == "C1":
            s.emit()
            return nc

        with ExitStack() as ph:
            skT = s.sb("skT_sb", [128, 16, 128], BF16, ph)
            s.dma("pool", skT[:], skT_d)
            hg = s.sb("hgc", [128, 16, 4, 128], BF16, ph)
            NGB = 4
            exgt = s.sb("exgt", [128, 2 * NGB, 2048], BF16, ph)
            qg = exgt[:, 0:4, :].rearrange("p a (b c) -> p (a b) c", c=512)
            EXGT_KEYS = [("ex", b, rr) for b in range(NGB) for rr in range(16)]
            S0t = s.sb("S0t", [128, 32, 128], F32, ph)
            S1a = s.sb("S1a", [128, 32, 128], F32, ph)
            T01 = s.sb("T01", [128, 2, 8, 16], F32, ph)
            wrk = s.sb("wrk", [128, 256], F32, ph)
            Ct = s.sb("Ct", [128, 8, 16], F32, ph)
            Ce = s.sb("Ce", [128, 8, 16], F32, ph)
            Zs = s.sb("Zs", [128, 8], F32, ph)
            en = s.sb("en", [128, 8], F32, ph)
            tau = s.sb("tau", [128, 8], F32, ph)
            Dg = s.sb("Dg", [128, 32, 128], BF16, ph)
            yacc = s.sb("yacc", [128, 4, D], F32, ph)
            cand_t = s.sb("cand", [128, 8, 256], F32, ph)
            cand = cand_t[:]
            dd1 = cand_t[:].rearrange("p a (c b) -> p (a c) b", c=2)
            ex = [exgt[:, i, :].rearrange("p (a b) -> p a b", a=16) for i in range(NGB)]
            Gt = [exgt[:, NGB + i, :].rearrange("p (a b) -> p a b", a=16) for i in range(NGB)]
            WTs = [s.sb("WTs%d" % i, [128, NB, 512], BF16, ph) for i in range(2)]
            gu = 0
            gel = s.sb("gel", [128, NB, 512], BF16, ph)
            WgT = s.sb("WgT", [128, NB, 512], BF16, ph)
            NU = 5
            UT = [s.sb("UT%d" % i, [128, 16, 128], BF16, ph) for i in range(NU)]
            Vc = [s.sb("Vc%d" % i, [128, D], BF16, ph) for i in range(2 * NB)]
            pAa = [s.ps("pAa%d" % i, [128, 512], F32, ph) for i in range(4)]
            pSc = pAa[0]
            pW = [s.ps("pW%d" % i, [128, 512], F32, ph) for i in range(2)]
            pY = [s.ps("pY%d" % i, [128, 512], F32, ph) for i in range(2)]
            for g in range(ngrp):
                s.dma("sp", hg[:], hn_scr[g], r=[("hn", 4 * g + i) for i in range(4)])
                s.dma("sp", qg, q_scr[g], r=[("q", g)], w=EXGT_KEYS)
                s.dma("sp", yacc[:], out_h[4 * g:4 * g + 4].rearrange("j p d -> p j d"), r=[("h", 4 * g + i) for i in range(4)])
                for tt in range(4):
                    for c0 in range(0, 16, 4):
                        for ci in range(4):
                            c = c0 + ci
                            s.op("pe", "matmul", out=pSc[:, ci * 128:(ci + 1) * 128], lhsT=qg[:, c, tt * 128:(tt + 1) * 128],
                                 rhs=skT[:, c, :], start=True, stop=True, r=EXGT_KEYS + [skT.name], w=[pSc.name])
                        for ci in range(4):
                            c = c0 + ci
                            h, half = c // 2, c % 2
                            dst = (S0t if half == 0 else S1a)[:, tt * 8 + h, :]
                            if (c0 // 4) % 2 == 0:
                                s.op("act", "copy", out=dst, in_=pSc[:, ci * 128:(ci + 1) * 128])
                            else:
                                s.op("dve", "tensor_copy", out=dst, in_=pSc[:, ci * 128:(ci + 1) * 128])
                    for half in range(2):
                        src = S0t if half == 0 else S1a
                        for h in range(8):
                            s.op("dve", "max", out=T01[:, half, h, 0:8], in_=src[:, tt * 8 + h, :])
                            s.op("dve", "match_replace", out=wrk[:, 0:128], in_to_replace=T01[:, half, h, 0:8],
                                 in_values=src[:, tt * 8 + h, :], imm_value=-1e30)
                            s.op("dve", "max", out=T01[:, half, h, 8:16], in_=wrk[:, 0:128])
                    s.op("dve", "tensor_tensor", out=cand[:].rearrange("p h (a b) -> p h a b", a=16),
                         in0=T01[:, 0, :, :].unsqueeze(3).to_broadcast([128, 8, 16, 16]),
                         in1=T01[:, 1, :, :].unsqueeze(2).to_broadcast([128, 8, 16, 16]), op=ALU.add)
                    for h in range(8):
                        s.op("dve", "max", out=Ct[:, h, 0:8], in_=cand[:, h, :])
                        s.op("dve", "match_replace", out=wrk[:], in_to_replace=Ct[:, h, 0:8], in_values=cand[:, h, :],
                             imm_value=-1e30)
                        s.op("dve", "max", out=Ct[:, h, 8:16], in_=wrk[:])
                    s.op("dve", "tensor_tensor", out=Ce[:], in0=Ct[:], in1=Ct[:, :, 0:1].to_broadcast([128, 8, 16]),
                         op=ALU.subtract)
                    s.op("act", "activation", out=Ce[:], in_=Ce[:], func=AF.Exp)
                    s.op("dve", "tensor_reduce", out=Zs[:], in_=Ce[:], axis=AX.X, op=ALU.add)
                    s.op("dve", "reciprocal", out=Zs[:], in_=Zs[:])
                    s.op("dve", "tensor_tensor", out=en[:], in0=Ce[:, :, 15], in1=Zs[:], op=ALU.mult)
                    s.op("dve", "tensor_scalar", out=tau[:], in0=Ct[:, :, 15], scalar1=-DELTA, scalar2=None, op0=ALU.add)
                    s.op("dve", "tensor_tensor", out=S0t[:, tt * 8:(tt + 1) * 8, :], in0=S0t[:, tt * 8:(tt + 1) * 8, :],
                         in1=tau[:].unsqueeze(2).to_broadcast([128, 8, 128]), op=ALU.subtract)
                    s.op("dve", "tensor_tensor", out=Dg[:, tt * 8:(tt + 1) * 8, :],
                         in0=ident[:].unsqueeze(1).to_broadcast([128, 8, 128]),
                         in1=en[:].unsqueeze(2).to_broadcast([128, 8, 128]), op=ALU.mult)
                    s.op("pool", "tensor_scalar", out=S1a[:, tt * 8:(tt + 1) * 8, :], in0=S1a[:, tt * 8:(tt + 1) * 8, :],
                         scalar1=-1.0, scalar2=None, op0=ALU.mult)
                    if g == 0 and tt == 0:
                        dbg("Ct", Ct[:], [128, 8, 16])
                        dbg("en", en[:], [128, 8])
                if upto == "C3a":
                    break
                def load_u(chunk):
                    s.dma("pool", UT[chunk % NU][:], ut_t[chunk])

                def load_v(blk_):
                    for i in range(NB):
                        n0_ = blk_ * NB + i
                        s.dma("pool", Vc[(blk_ % 2) * NB + i][:], pv[n0_ * 128:(n0_ + 1) * 128, :])

                def gate_ops(u):
                    b = u % NGB
                    n0 = u // 2
                    hh = u % 2
                    exkeys = [("ex", b, rr) for rr in range(16)]
                    for rr in range(16):
                        row = 16 * hh + rr
                        s.op("act", "activation", out=ex[b][:, rr, :], in_=S1a[:, row, :], func=AF.Exp, scale=-1.0,
                             bias=S0t[:, row, n0:n0 + 1], r=[S1a.name, S0t.name], w=[("ex", b, rr)])
                    s.op("dve", "tensor_tensor", out=Gt[b], in0=S1a[:, 16 * hh:16 * hh + 16, :],
                         in1=S0t[:, 16 * hh:16 * hh + 16, n0:n0 + 1].to_broadcast([128, 16, 128]), op=ALU.is_le,
                         r=[S1a.name, S0t.name], w=[("Gt", b)])
                    s.op("dve", "tensor_tensor", out=Gt[b], in0=Gt[b], in1=ex[b], op=ALU.mult,
                         r=[("Gt", b)] + exkeys, w=[("Gt", b)])

                def gate_mm(u):
                    b = u % NGB
                    n0 = u // 2
                    hh = u % 2
                    blk_, i = n0 // NB, n0 % NB
                    pw_ = pW[n0 % 2]
                    for t2 in range(2):
                        tt = 2 * hh + t2
                        for h in range(8):
                            s.op("pe", "matmul", out=pw_[:, tt * 128:(tt + 1) * 128], lhsT=Gt[b][:, t2 * 8 + h, :],
                                 rhs=Dg[:, tt * 8 + h, :], start=(h == 0), stop=(h == 7),
                                 r=[("Gt", b), Dg.name], w=[pw_.name])
                    if hh == 1:
                        s.op("dve", "tensor_copy", out=WTs[blk_ % 2][:, i, :], in_=pw_[:])

                def a_mm(blk_, i):
                    chunk = blk_ * NB + i
                    pa_ = pAa[chunk % 4]
                    for kc in range(16):
                        s.op("pe", "matmul", out=pa_[:], lhsT=UT[chunk % NU][:, kc, :],
                             rhs=hg[:, kc, :, :].rearrange("p a b -> p (a b)"), start=(kc == 0), stop=(kc == 15))

                def a_post(blk_, i):
                    pa_ = pAa[(blk_ * NB + i) % 4]
                    s.op("act", "activation", out=gel[:, i, :], in_=pa_[:], func=AF.Gelu_apprx_tanh)

                def a_mult(blk_, i):
                    s.op("dve", "tensor_tensor", out=WgT[:, i, :], in0=WTs[blk_ % 2][:, i, :], in1=gel[:, i, :], op=ALU.mult)

                yctr = [0]

                def y_items(blk_, q):
                    tt = q
                    for dg in range(4):
                        py_ = pY[yctr[0] % 2]
                        yctr[0] += 1
                        for i in range(NB):
                            s.op("pe", "matmul", out=py_[:], lhsT=WgT[:, i, tt * 128:(tt + 1) * 128],
                                 rhs=Vc[(blk_ % 2) * NB + i][:, dg * 512:(dg + 1) * 512], start=(i == 0), stop=(i == NB - 1))
                        s.op("dve", "tensor_tensor", out=yacc[:, tt, dg * 512:(dg + 1) * 512],
                             in0=py_[:], in1=yacc[:, tt, dg * 512:(dg + 1) * 512], op=ALU.add)

                NUN = nblk * 2 * NB
                for c in range(min(NU, nblk * NB)):
                    load_u(c)
                load_v(0)
                nu_loaded = min(NU, nblk * NB)
                st = {"gops": 0, "gmm": 0}

                def pump(limit, force_mm=False):
                    if st["gmm"] < min(limit, st["gops"]) and (force_mm or st["gops"] - st["gmm"] >= min(NGB, limit - st["gmm"])):
                        gate_mm(st["gmm"])
                        st["gmm"] += 1
                    while st["gops"] < limit and st["gops"] - st["gmm"] < NGB:
                        gate_ops(st["gops"])
                        st["gops"] += 1

                lim0 = min(NUN, 2 * NB)
                while st["gmm"] < lim0:
                    pump(lim0, force_mm=True)
                for blk in range(nblk):
                    lim = min(NUN, (blk + 2) * 2 * NB)
                    if blk + 1 < nblk:
                        load_v(blk + 1)
                    for k in range(2 * NB):
                        if k < NB:
                            a_mm(blk, k)
                            if k == NB - 1:
                                for i_ in range(NB):
                                    a_post(blk, i_)
                                for i_ in range(NB):
                                    a_mult(blk, i_)
                            if nu_loaded < nblk * NB and nu_loaded - NU <= blk * NB + k:
                                load_u(nu_loaded)
                                nu_loaded += 1
                        else:
                            y_items(blk, k - NB)
                        pump(lim)
                    while st["gmm"] < lim:
                        pump(lim, force_mm=True)
                s.dma("sp", out_h[4 * g:4 * g + 4].rearrange("j p d -> p j d"), yacc[:], w=[("h", 4 * g + i) for i in range(4)])
        s.emit()
    return nc


_NC_CACHE = {}


def _layout_inputs(inp):
    x = np.asarray(inp["x"], dtype=np.float32)
    pos = np.asarray(inp["positions"], dtype=np.int32)
    half = 32
    invf = (10000.0 ** (-np.arange(half, dtype=np.float32) / half)).astype(np.float32)
    invf_t = np.ascontiguousarray(np.broadcast_to(invf, (128, 32))).astype(np.float32)
    U = np.asarray(inp["peer_u"], dtype=np.float32)[0]
    ut_t = np.ascontiguousarray(U.reshape(NCHUNK, 128, 16, 128).transpose(0, 3, 2, 1))
    sk = np.asarray(inp["peer_sub_keys"], dtype=np.float32)[0]
    skT = np.ascontiguousarray(sk.reshape(16, 128, 128).transpose(2, 0, 1))
    shared = {
        "invf": invf_t,
        "norm1_gain": np.ascontiguousarray(inp["norm1_gain"][0], dtype=np.float32),
        "w_in": np.ascontiguousarray(inp["w_in"][0], dtype=np.float32),
        "q_a_gain": np.ascontiguousarray(inp["q_a_gain"][0], dtype=np.float32),
        "w_q_b": np.ascontiguousarray(inp["w_q_b"][0], dtype=np.float32),
        "kv_a_gain": np.ascontiguousarray(inp["kv_a_gain"][0], dtype=np.float32),
        "w_kv_b": np.ascontiguousarray(inp["w_kv_b"][0], dtype=np.float32),
        "mla_q_gain": np.ascontiguousarray(inp["mla_q_gain"][0], dtype=np.float32),
        "mla_k_gain": np.ascontiguousarray(inp["mla_k_gain"][0], dtype=np.float32),
        "swa_q_gain": np.ascontiguousarray(inp["swa_q_gain"][0], dtype=np.float32),
        "swa_k_gain": np.ascontiguousarray(inp["swa_k_gain"][0], dtype=np.float32),
        "swa_sinks": np.ascontiguousarray(inp["swa_sinks"][0], dtype=np.float32),
        "rel_bias_table": np.ascontiguousarray(np.asarray(inp["rel_bias_table"], dtype=np.float32).reshape(-1)),
        "group_out_gain": np.ascontiguousarray(inp["group_out_gain"][0], dtype=np.float32),
        "w_out": np.ascontiguousarray(inp["w_out"][0], dtype=np.float32),
        "norm2_gain": np.ascontiguousarray(inp["norm2_gain"][0], dtype=np.float32),
        "peer_w_q": np.ascontiguousarray(inp["peer_w_q"][0], dtype=np.float32),
        "skT": skT,
        "ut_t": ut_t,
        "peer_v": np.ascontiguousarray(inp["peer_v"][0], dtype=np.float32),
    }
    in_maps = []
    for c in range(8):
        b, p = c // 2, c % 2
        xb = x[b].reshape(NS, 128, D)
        pb = pos[b].reshape(NS, 128)
        order = []
        for j in range(NT):
            order += [2 * j + p, 2 * j + 1 - p]
        order = np.array(order)
        m = dict(shared)
        m["x_s"] = np.ascontiguousarray(xb[order])
        ps_ = pb[order]
        m["pos_r"] = np.ascontiguousarray(ps_)
        m["pos_s"] = np.ascontiguousarray(ps_.T)
        fl = np.zeros((128, 2), np.float32)
        fl[:, 0] = 1.0 if p == 1 else 0.0
        fl[:, 1] = 1.0 if p == 0 else 0.0
        m["flags"] = fl
        in_maps.append(m)
    return in_maps


def _assemble(results, key="out"):
    out = np.zeros((4, 4096, D), np.float32)
    ov = out.reshape(4, NS, 128, D)
    for c in range(8):
        b, p = c // 2, c % 2
        r = np.asarray(results[c][key]).reshape(NT, 128, D)
        for j in range(NT):
            ov[b, 2 * j + p] = r[j]
    return out


def kernel(**inputs):
    if "nc" not in _NC_CACHE:
        _NC_CACHE["nc"] = build_program()
    nc = _NC_CACHE["nc"]
    in_maps = _layout_inputs(inputs)
    res = run_bass_kernel_spmd(nc, in_maps, core_ids=list(range(8)))
    return _assemble(res.results)
```
